# Optimizing a Trainium2 kernel written in Bass

```python
import math
import jax, jax.numpy as jnp
from jax import lax
import numpy as np

D_MODEL = 1024
BATCH = 2
SEQ = 8192
DEPTH = 4

ATTN_HEADS = 8
ATTN_HEAD_DIM = 64
ATTN_QK_WIDTH = 2 * ATTN_HEADS * ATTN_HEAD_DIM
ATTN_V_WIDTH = ATTN_HEADS * 2 * ATTN_HEAD_DIM
Q_BLOCK = 128
GMLP_CHUNK = 128
GMLP_GROUPS = 8
GMLP_GROUP_DIM = 128
GMLP_WIDTH = GMLP_GROUPS * GMLP_GROUP_DIM
IN_WIDTH = 2 * ATTN_QK_WIDTH + ATTN_V_WIDTH + 2 * GMLP_WIDTH + 2 * D_MODEL
N_EXPERTS = 16
EXPERT_FF = 2 * D_MODEL
EC_CAPACITY = 2
NORM_EPS = 1e-6

kernel_name = 'hybrid_diffattn_gmlp_ecmoe_encoder'


def _rmsnorm(x, g):
    x32 = x.astype(jnp.float32)
    y = x32 * lax.rsqrt(jnp.mean(x32 * x32, axis=-1, keepdims=True) + NORM_EPS)
    return (y * g.astype(jnp.float32)).astype(x.dtype)


def _layernorm(x, g, b):
    x32 = x.astype(jnp.float32)
    mu = jnp.mean(x32, axis=-1, keepdims=True)
    xc = x32 - mu
    var = jnp.mean(xc * xc, axis=-1, keepdims=True)
    y = xc * lax.rsqrt(var + NORM_EPS) * g.astype(jnp.float32) + b.astype(jnp.float32)
    return y.astype(x.dtype)


def _alibi_slopes():
    return 2.0 ** (-8.0 * jnp.arange(1, ATTN_HEADS + 1, dtype=jnp.float32) / ATTN_HEADS)


def _diff_lambda(lq1, lk1, lq2, lk2, lam_init):
    f = lambda a: a.astype(jnp.float32)
    return jnp.exp(jnp.sum(f(lq1) * f(lk1))) - jnp.exp(jnp.sum(f(lq2) * f(lk2))) + lam_init


def _diff_attention(q, k, v, lam):
    B, S = q.shape[0], q.shape[1]
    nb = S // Q_BLOCK
    qb = jnp.moveaxis(q.reshape(B, nb, Q_BLOCK, 2, ATTN_HEADS, ATTN_HEAD_DIM), 1, 0)
    kpos = jnp.arange(S)
    slopes = _alibi_slopes()
    scale = ATTN_HEAD_DIM ** -0.5

    def block(args):
        qblk, i = args
        qpos = i * Q_BLOCK + jnp.arange(Q_BLOCK)
        dist = jnp.abs(qpos[:, None] - kpos[None, :]).astype(jnp.float32)
        bias = -slopes[:, None, None] * dist
        s = jnp.einsum('bqnhd,bknhd->bnhqk', qblk, k).astype(jnp.float32) * scale + bias
        p = jax.nn.softmax(s, axis=-1)
        a = p[:, 0] - lam * p[:, 1]
        return jnp.einsum('bhqk,bkhe->bqhe', a.astype(v.dtype), v)

    out = lax.map(block, (qb, jnp.arange(nb)))
    return jnp.moveaxis(out, 0, 1).reshape(B, S, ATTN_HEADS, 2 * ATTN_HEAD_DIM)


def _spatial_gating(u, vg, ln_g, ln_b, w_s, b_s):
    u = jax.nn.gelu(u, approximate=False)
    vg = _layernorm(jax.nn.gelu(vg, approximate=False), ln_g, ln_b)
    B, S, W = vg.shape
    vc = vg.reshape(B, S // GMLP_CHUNK, GMLP_CHUNK, GMLP_GROUPS, GMLP_GROUP_DIM)
    mixed = jnp.einsum('gts,bcsgk->bctgk', w_s, vc) + jnp.swapaxes(b_s, 0, 1)[None, None, :, :, None]
    return u * mixed.reshape(B, S, W)


def _mixer(h, w_in, lq1, lk1, lq2, lk2, g_subln, ln_v_g, ln_v_b, w_s, b_s, w_a, w_b, w_o, lam_init):
    B, S, _ = h.shape
    z = h @ w_in
    cuts = np.cumsum([ATTN_QK_WIDTH, ATTN_QK_WIDTH, ATTN_V_WIDTH, GMLP_WIDTH, GMLP_WIDTH, D_MODEL]).tolist()
    q, k, v, u, vg, ga, gb = jnp.split(z, cuts, axis=-1)
    q = q.reshape(B, S, 2, ATTN_HEADS, ATTN_HEAD_DIM)
    k = k.reshape(B, S, 2, ATTN_HEADS, ATTN_HEAD_DIM)
    v = v.reshape(B, S, ATTN_HEADS, 2 * ATTN_HEAD_DIM)
    lam = _diff_lambda(lq1, lk1, lq2, lk2, lam_init)
    o = _diff_attention(q, k, v, lam)
    o = _rmsnorm(o, g_subln) * (1.0 - lam_init)
    ya = o.reshape(B, S, ATTN_V_WIDTH) @ w_a
    yb = _spatial_gating(u, vg, ln_v_g, ln_v_b, w_s, b_s) @ w_b
    merged = jax.nn.sigmoid(ga) * ya + jax.nn.sigmoid(gb) * yb
    return merged @ w_o


def _expert_choice_moe(h, w_r, b_r, w_g, w_u, w_d):
    B, S, _ = h.shape
    cap = EC_CAPACITY * S // N_EXPERTS
    logits = (h @ w_r + b_r).astype(jnp.float32)
    aff = jax.nn.softmax(logits, axis=-1)
    gate, idx = lax.top_k(jnp.swapaxes(aff, 1, 2), cap)
    bidx = jnp.arange(B)[:, None, None]
    xg = h[bidx, idx]
    hid = jax.nn.silu(jnp.einsum('becd,edf->becf', xg, w_g)) * jnp.einsum('becd,edf->becf', xg, w_u)
    y = jnp.einsum('becf,efd->becd', hid, w_d) * gate[..., None].astype(h.dtype)
    return jnp.zeros_like(h).at[bidx, idx].add(y)


def _modulate(x, g, shift, scale):
    return _rmsnorm(x, g) * (1.0 + scale[:, None, :]) + shift[:, None, :]


def setup_inputs(seed: int = 0) -> dict:
    key = jax.random.key(seed)
    ks = jax.random.split(key, 26)
    L = DEPTH

    def nrm(k, shape, s):
        return jax.random.normal(k, shape, jnp.float32) * s

    return {
        'x': nrm(ks[0], (BATCH, SEQ, D_MODEL), 1.0),
        'c': nrm(ks[1], (BATCH, D_MODEL), 1.0),
        'w_ada': nrm(ks[2], (L, D_MODEL, 6 * D_MODEL), 0.25 * D_MODEL ** -0.5),
        'b_ada': nrm(ks[3], (L, 6 * D_MODEL), 0.01),
        'g_pre_mix': 1.0 + nrm(ks[4], (L, D_MODEL), 0.05),
        'g_post_mix': 1.0 + nrm(ks[5], (L, D_MODEL), 0.05),
        'w_in': nrm(ks[6], (L, D_MODEL, IN_WIDTH), D_MODEL ** -0.5),
        'lam_q1': nrm(ks[7], (L, ATTN_HEAD_DIM), 0.1),
        'lam_k1': nrm(ks[8], (L, ATTN_HEAD_DIM), 0.1),
        'lam_q2': nrm(ks[9], (L, ATTN_HEAD_DIM), 0.1),
        'lam_k2': nrm(ks[10], (L, ATTN_HEAD_DIM), 0.1),
        'g_subln': 1.0 + nrm(ks[11], (L, 2 * ATTN_HEAD_DIM), 0.05),
        'ln_v_g': 1.0 + nrm(ks[12], (L, GMLP_WIDTH), 0.05),
        'ln_v_b': nrm(ks[13], (L, GMLP_WIDTH), 0.01),
        'w_spatial': nrm(ks[14], (L, GMLP_GROUPS, GMLP_CHUNK, GMLP_CHUNK), GMLP_CHUNK ** -0.5),
        'b_spatial': 1.0 + nrm(ks[15], (L, GMLP_GROUPS, GMLP_CHUNK), 0.01),
        'w_branch_a': nrm(ks[16], (L, ATTN_V_WIDTH, D_MODEL), ATTN_V_WIDTH ** -0.5),
        'w_branch_b': nrm(ks[17], (L, GMLP_WIDTH, D_MODEL), GMLP_WIDTH ** -0.5),
        'w_out': nrm(ks[18], (L, D_MODEL, D_MODEL), D_MODEL ** -0.5),
        'g_pre_ffn': 1.0 + nrm(ks[19], (L, D_MODEL), 0.05),
        'g_post_ffn': 1.0 + nrm(ks[20], (L, D_MODEL), 0.05),
        'w_router': nrm(ks[21], (L, D_MODEL, N_EXPERTS), D_MODEL ** -0.5),
        'b_router': nrm(ks[22], (L, N_EXPERTS), 0.01),
        'w_gate_e': nrm(ks[23], (L, N_EXPERTS, D_MODEL, EXPERT_FF), D_MODEL ** -0.5),
        'w_up_e': nrm(ks[24], (L, N_EXPERTS, D_MODEL, EXPERT_FF), D_MODEL ** -0.5),
        'w_down_e': nrm(ks[25], (L, N_EXPERTS, EXPERT_FF, D_MODEL), EXPERT_FF ** -0.5),
    }


def reference(x, c, w_ada, b_ada, g_pre_mix, g_post_mix, w_in, lam_q1, lam_k1, lam_q2, lam_k2,
              g_subln, ln_v_g, ln_v_b, w_spatial, b_spatial, w_branch_a, w_branch_b, w_out,
              g_pre_ffn, g_post_ffn, w_router, b_router, w_gate_e, w_up_e, w_down_e):
    c_act = jax.nn.silu(c)
    for l in range(DEPTH):
        lam_init = 0.8 - 0.6 * math.exp(-0.3 * l)
        mod = c_act @ w_ada[l] + b_ada[l]
        sh1, sc1, gt1, sh2, sc2, gt2 = jnp.split(mod, 6, axis=-1)
        h = _modulate(x, g_pre_mix[l], sh1, sc1)
        y = _mixer(h, w_in[l], lam_q1[l], lam_k1[l], lam_q2[l], lam_k2[l], g_subln[l],
                   ln_v_g[l], ln_v_b[l], w_spatial[l], b_spatial[l],
                   w_branch_a[l], w_branch_b[l], w_out[l], lam_init)
        x = x + gt1[:, None, :] * _rmsnorm(y, g_post_mix[l])
        h = _modulate(x, g_pre_ffn[l], sh2, sc2)
        y = _expert_choice_moe(h, w_router[l], b_router[l], w_gate_e[l], w_up_e[l], w_down_e[l])
        x = x + gt2[:, None, :] * _rmsnorm(y, g_post_ffn[l])
    return x
```

```python
import math
from contextlib import ExitStack

import numpy as np
import concourse.bass as bass
import concourse.mybir as mybir
from concourse.bass_utils import run_bass_kernel_spmd

F32 = mybir.dt.float32
BF16 = mybir.dt.bfloat16
I32 = mybir.dt.int32
AF = mybir.ActivationFunctionType
ALU = mybir.AluOpType
AX = mybir.AxisListType

D = 1024
SEQ = 8192
TOK = 2048
NT = 16
NH = 8
NE = 16
EOWN = 4
CAP = 1024
FF = 2048
EPS = 1e-6
NBIS = 26
BIG = 1.0e6


class Op:
    __slots__ = ("eng", "fn", "kind", "inc", "seq", "sem", "val", "count")

    def __init__(self, eng, fn, kind):
        self.eng = eng
        self.fn = fn
        self.kind = kind
        self.inc = False
        self.seq = -1
        self.sem = None
        self.val = 0
        self.count = 0


ENGS = ("pe", "act", "dve", "pool", "sp")


class Prog:
    def __init__(self, nc, es):
        self.nc = nc
        self.items = {e: [] for e in ENGS}
        self.nseq = {e: 0 for e in ENGS}
        self.lastw = {}
        self.rd_c = {}
        self.rd_d = {}
        self.waited = {e: {} for e in ENGS}
        self.waited_d = {e: set() for e in ENGS}
        self.last_c = {e: None for e in ENGS}
        self.pending_d = []
        self.esem = {e: es.enter_context(nc.semaphore("s_" + e)) for e in ENGS}
        ndma = {"sp": 24, "pool": 40, "act": 2}
        self.dsem = {e: [es.enter_context(nc.semaphore("d_%s%d" % (e, i))) for i in range(n)]
                     for e, n in ndma.items()}
        self.dsem_use = {e: [0] * n for e, n in ndma.items()}
        self.dsem_last = {e: [None] * n for e, n in ndma.items()}
        self.dsem_next = {e: 0 for e in ndma}
        self.csem = es.enter_context(nc.semaphore("cc"))
        self.csem_use = 0

    def _need(self, eng, d):
        if d.kind != "c":
            if id(d) in self.waited_d[eng]:
                return
            self.waited_d[eng].add(id(d))
        else:
            if d.eng == eng and eng == "pe":
                return
            if self.waited[eng].get(d.eng, -1) >= d.seq:
                return
            self.waited[eng][d.eng] = d.seq
            d.inc = True
        self.items[eng].append(("w", d))

    def op(self, eng, fn, r=(), w=(), kind="c"):
        o = Op(eng, fn, kind)
        for k in r:
            lw = self.lastw.get(k)
            if lw is not None:
                self._need(eng, lw)
        for k in w:
            lw = self.lastw.get(k)
            if lw is not None:
                self._need(eng, lw)
            for ro in self.rd_c.get(k, {}).values():
                self._need(eng, ro)
            for ro in self.rd_d.get(k, ()):
                self._need(eng, ro)
        if kind == "d":
            i = self.dsem_next[eng]
            self.dsem_next[eng] = (i + 1) % len(self.dsem[eng])
            prev = self.dsem_last[eng][i]
            if prev is not None:
                self._need(eng, prev)
            self.dsem_use[eng][i] += 1
            o.sem = self.dsem[eng][i]
            o.val = 16 * self.dsem_use[eng][i]
            self.dsem_last[eng][i] = o
            self.pending_d.append(o)
        elif kind == "x":
            self.csem_use += 1
            o.sem = self.csem
            o.val = self.csem_use
            self.pending_d.append(o)
        else:
            o.seq = self.nseq[eng]
            self.nseq[eng] += 1
            self.last_c[eng] = o
        self.items[eng].append(o)
        for k in r:
            if kind == "c":
                self.rd_c.setdefault(k, {})[eng] = o
            else:
                self.rd_d.setdefault(k, []).append(o)
        for k in w:
            self.lastw[k] = o
            self.rd_c[k] = {}
            self.rd_d[k] = []
        return o

    def barrier(self):
        latest = {}
        for d in self.pending_d:
            latest[id(d.sem)] = d
        for d in latest.values():
            self._need("sp", d)
        for e2 in ENGS:
            if e2 != "sp" and self.last_c[e2] is not None:
                self._need("sp", self.last_c[e2])
        b = Op("sp", lambda e: e.nop(), "c")
        b.seq = self.nseq["sp"]
        self.nseq["sp"] += 1
        self.last_c["sp"] = b
        self.items["sp"].append(b)
        for e in ENGS:
            if e != "sp":
                self._need(e, b)
        self.pending_d = []
        self.lastw = {}
        self.rd_c = {}
        self.rd_d = {}

    def emit(self, block):
        for e in ENGS:
            c = 0
            for it in self.items[e]:
                if isinstance(it, Op) and it.kind == "c":
                    if it.inc:
                        c += 1
                    it.count = c
        esem = self.esem

        def run(e_name):
            def body(e):
                for it in self.items[e_name]:
                    if isinstance(it, Op):
                        ins = it.fn(e)
                        if it.kind == "d":
                            ins.then_inc(it.sem, 16)
                        elif it.kind == "x":
                            ins.then_inc(it.sem, 1)
                        elif it.inc:
                            ins.then_inc(esem[e_name], 1)
                    else:
                        d = it[1]
                        if d.kind == "c":
                            e.wait_ge(esem[d.eng], d.count)
                        else:
                            e.wait_ge(d.sem, d.val)
            return body

        block.tensor(run("pe"))
        block.scalar(run("act"))
        block.vector(run("dve"))
        block.gpsimd(run("pool"))
        block.sync(run("sp"))

    def mm(self, out, lhsT, rhs, start=True, stop=True, r=(), w=()):
        return self.op("pe", lambda e: e.matmul(out, lhsT=lhsT, rhs=rhs, start=start, stop=stop), r, w)

    def tr(self, out, in_, ident, r=(), w=()):
        return self.op("pe", lambda e: e.transpose(out, in_, ident), r, w)

    def act(self, out, in_, func, r=(), w=(), bias=None, scale=None, accum_out=None):
        kw = {}
        if bias is not None:
            kw["bias"] = bias
        if scale is not None:
            kw["scale"] = scale
        if accum_out is not None:
            kw["accum_out"] = accum_out
        return self.op("act", lambda e: e.activation(out=out, in_=in_, func=func, **kw), r, w)

    def tt(self, eng, out, in0, in1, op, r=(), w=()):
        return self.op(eng, lambda e: e.tensor_tensor(out=out, in0=in0, in1=in1, op=op), r, w)

    def ts(self, eng, out, in0, s1, op0, s2=None, op1=None, r=(), w=()):
        if op1 is None:
            return self.op(eng, lambda e: e.tensor_scalar(out=out, in0=in0, scalar1=s1, scalar2=None, op0=op0), r, w)
        return self.op(eng, lambda e: e.tensor_scalar(out=out, in0=in0, scalar1=s1, scalar2=s2, op0=op0, op1=op1), r, w)

    def stt(self, eng, out, in0, scalar, in1, op0, op1, r=(), w=()):
        return self.op(eng, lambda e: e.scalar_tensor_tensor(out=out, in0=in0, scalar=scalar, in1=in1, op0=op0, op1=op1), r, w)

    def cp(self, eng, out, in_, r=(), w=()):
        if eng == "act":
            return self.op(eng, lambda e: e.copy(out=out, in_=in_), r, w)
        return self.op(eng, lambda e: e.tensor_copy(out, in_), r, w)

    def red(self, eng, out, in_, op, r=(), w=()):
        return self.op(eng, lambda e: e.tensor_reduce(out=out, in_=in_, axis=AX.X, op=op), r, w)

    def memset(self, eng, ap, val, r=(), w=()):
        return self.op(eng, lambda e: e.memset(ap, val), r, w)

    def dma(self, eng, out, in_, r=(), w=()):
        return self.op(eng, lambda e: e.dma_start(out=out, in_=in_), r, w, kind="d")

    def _breg(self, e, bound):
        if not hasattr(self, "bregs"):
            self.bregs = {}
        if bound not in self.bregs:
            rg = e.alloc_register(name="bnd%d" % bound)
            e.reg_mov(rg, bound)
            self.bregs[bound] = rg
        return self.bregs[bound]

    def gather(self, out, src, idx, bound, r=(), w=()):
        return self.op("pool", lambda e: e.indirect_dma_start(
            out=out, out_offset=None, in_=src,
            in_offset=bass.IndirectOffsetOnAxis(ap=idx, axis=0),
            bounds_check=self._breg(e, bound), oob_is_err=False), r, w, kind="d")

    def scatter(self, dst, idx, in_, bound, r=(), w=()):
        return self.op("pool", lambda e: e.indirect_dma_start(
            out=dst, out_offset=bass.IndirectOffsetOnAxis(ap=idx, axis=0),
            in_=in_, in_offset=None,
            bounds_check=self._breg(e, bound), oob_is_err=False), r, w, kind="d")

    def allgather(self, out, in_, r=(), w=()):
        return self.op("pool", lambda e: e.collective_compute(
            "AllGather", ALU.bypass, replica_groups=[[0, 1, 2, 3], [4, 5, 6, 7]],
            ins=[in_], outs=[out]), r, w, kind="x")


def build_program(L, lam_inits, dbg=None, phases=None, n_own=EOWN, final=True):
    dbg = dbg or set()
    allph = {"mix_a", "attn", "mix_b", "moe_r", "moe_e", "moe_c"}
    phases = allph if phases is None else phases
    nc = bass.Bass("TRN2", target_bir_lowering=False)

    def din(name, shape, dt=F32):
        return nc.dram_tensor(name, list(shape), dt, kind="ExternalInput").ap()

    def dint(name, shape, dt=F32):
        return nc.dram_tensor(name, list(shape), dt, kind="Internal").ap()

    def dout(name, shape, dt=F32):
        return nc.dram_tensor(name, list(shape), dt, kind="ExternalOutput").ap()

    x_in = din("x", [TOK, D])
    c_pk = din("c_pk", [128, 8])
    w_ada_s = din("w_ada_s", [L, 256, 6 * D])
    w_in_s = din("w_in_s", [L, 256, 7 * D])
    w_a_s = din("w_a_s", [L, 256, D])
    w_b_s = din("w_b_s", [L, 256, D])
    w_o_s = din("w_o_s", [L, 256, D])
    b_ada = din("b_ada", [L, 6 * D])
    g_pre_mix = din("g_pre_mix", [L, D])
    g_post_mix = din("g_post_mix", [L, D])
    g_pre_ffn = din("g_pre_ffn", [L, D])
    g_post_ffn = din("g_post_ffn", [L, D])
    ln_v_g = din("ln_v_g", [L, D])
    ln_v_b = din("ln_v_b", [L, D])
    g_subln = din("g_subln", [L, 128])
    lamv = din("lamv", [L, 4, 64])
    lamc = din("lamc", [128, 2 * L])
    wsT = din("wsT", [L, 128, 8, 128])
    b_sp = din("b_sp", [L, 8 * 128])
    w_r = din("w_r", [L, D, NE])
    b_r = din("b_r", [L, NE])
    if "moe_e" in phases:
        w_ge = din("w_ge", [L, n_own, D, FF])
        w_ue = din("w_ue", [L, n_own, D, FF])
        w_de = din("w_de", [L, n_own, FF, D])
    ident = din("ident", [128, 128])
    utri = din("utri", [128, 128])
    qaug = din("qaug", [NH, 3, 5, TOK])
    kaug = din("kaug", [NH, 5, SEQ])
    bdiag = din("bdiag", [128, NH, 128])
    idxK = din("idxK", [64, 64], I32)
    idxV = din("idxV", [128, 64], I32)
    idxOwn = din("idxOwn", [128, NT], I32)
    selb = din("selb", [128, EOWN, 64 * NE])

    out = dout("out", [TOK, D])
    taps = {}

    def tap(name, shape, dt=F32):
        if name in dbg:
            taps[name] = dout("t_" + name, shape, dt)
            return taps[name]
        return None

    W_ADA = dint("W_ADA", [L, D, 6 * D])
    W_IN = dint("W_IN", [L, D, 7 * D])
    W_A = dint("W_A", [L, D, D])
    W_B = dint("W_B", [L, D, D])
    W_O = dint("W_O", [L, D, D])
    B_ADA = dint("B_ADA", [L, 256, 6 * D])
    B_IN = dint("B_IN", [L, 256, 7 * D])
    B_A = dint("B_A", [L, 256, D])
    B_B = dint("B_B", [L, 256, D])
    B_O = dint("B_O", [L, 256, D])
    XC = dint("XC", [TOK, D])
    QT = dint("QT", [D, TOK], BF16)
    KT_loc = dint("KT_loc", [D, TOK], BF16)
    KT_all = dint("KT_all", [4 * D, TOK], BF16)
    V_loc = dint("V_loc", [NH, TOK, 128], BF16)
    V_all = [dint("V_all%d" % h, [SEQ, 128], BF16) for h in range(NH)]
    SGA = dint("SGA", [D, TOK])
    SGB = dint("SGB", [D, TOK])
    YB = dint("YB", [D, TOK])
    OT = dint("OT", [D, TOK], BF16)
    H2_loc = dint("H2_loc", [TOK, D])
    H2_all = dint("H2_all", [SEQ, D])
    AFF_loc = dint("AFF_loc", [TOK, NE])
    AFF_all = dint("AFF_all", [SEQ, NE])
    XG = dint("XG", [EOWN * CAP, D])
    Y_loc = dint("Y_loc", [EOWN * CAP, D])
    Y_all = dint("Y_all", [NE * CAP, D])
    IDXF = dint("IDXF", [SEQ, 2 * NE])

    es = ExitStack()
    with es:
        P = Prog(nc, es)

        def sb(name, shape, dt=F32):
            return es.enter_context(nc.sbuf_tensor(name, list(shape), dt))

        ident_f = sb("ident_f", [128, 128])
        ident_b = sb("ident_b", [128, 128], BF16)
        ut_f = sb("ut_f", [128, 128])
        ones_f = sb("ones_f", [128, 128])
        epsb = sb("epsb", [128, 1])
        bdg = sb("bdg", [128, NH, 128], BF16)
        idxK_s = sb("idxK_s", [64, 64], I32)
        idxV_s = sb("idxV_s", [128, 64], I32)
        idxO_s = sb("idxO_s", [128, NT], I32)
        lamc_s = sb("lamc_s", [128, 2 * L])
        cact = sb("cact", [128, 8])
        vec = sb("vec", [128, 6, D])
        gsc = sb("gsc", [128, 1])
        nlam = sb("nlam", [128, 1])
        lamt = sb("lamt", [128, 4, 64])
        lamr = sb("lamr", [128, 4])
        aff_own = sb("aff_own", [128, NT, NE])
        wr_s = sb("wr_s", [128, 8, NE])
        br_s = sb("br_s", [128, NE])
        wring = [sb("wring%d" % i, [128, 8 * D], BF16) for i in range(2)]
        ARENA = 35840
        arena = sb("arena", [128, ARENA])
        ps = [es.enter_context(nc.psum_tensor("ps%d" % i, [128, 2, 512], F32)) for i in range(4)]

        class Carver:
            def __init__(self):
                self.off = 0

            def f32(self, n, shape=None):
                a = arena[:, self.off:self.off + n]
                self.off += n
                assert self.off <= ARENA, self.off
                if shape is not None:
                    a = a.rearrange(shape[0], **shape[1])
                return a

            def bf(self, n, shape=None):
                w = (n + 1) // 2
                a = arena[:, self.off:self.off + w].bitcast(BF16)
                self.off += w
                assert self.off <= ARENA, self.off
                if shape is not None:
                    a = a.rearrange(shape[0], **shape[1])
                return a

            def i32(self, n):
                a = arena[:, self.off:self.off + n].bitcast(I32)
                self.off += n
                assert self.off <= ARENA, self.off
                return a

        def psb(i, j):
            return ps[i][:, j, :]

        def psbf(i, j):
            return ps[i][:, j, :].bitcast(BF16)

        P.dma("sp", ident_f[:], ident, w=["ident_f"])
        P.dma("pool", ident_b[:], ident, w=["ident_b"])
        P.dma("sp", ut_f[:], utri, w=["ut_f"])
        P.dma("pool", bdg[:], bdiag, w=["bdg"])
        P.dma("sp", idxK_s[:], idxK, w=["idxK"])
        P.dma("sp", idxV_s[:], idxV, w=["idxV"])
        P.dma("sp", idxO_s[:], idxOwn, w=["idxO"])
        P.dma("sp", lamc_s[:], lamc, w=["lamc"])
        P.memset("dve", ones_f[:], 1.0, w=["ones_f"])
        P.memset("dve", epsb[:], EPS, w=["epsb"])
        P.dma("sp", cact[:], c_pk, w=["cact"])
        P.act(cact[:], cact[:], AF.Silu, r=["cact"], w=["cact"])
        P.dma("sp", XC, x_in, w=["XC"])
        def load_shared(l, only=None):
            specs = (("W_IN", W_IN, w_in_s, B_IN), ("W_ADA", W_ADA, w_ada_s, B_ADA), ("W_A", W_A, w_a_s, B_A),
                     ("W_B", W_B, w_b_s, B_B), ("W_O", W_O, w_o_s, B_O))
            if only is not None:
                specs = specs[only:only + 1]
            for nm, dst_, src_, bnc in specs:
                P.dma("sp", bnc[l], src_[l], w=["b%s%d" % (nm, l)])
            for nm, dst_, src_, bnc in specs:
                if nm in ("W_IN", "W_ADA"):
                    for p in range(8):
                        P.allgather(dst_[l][p * 128:(p + 1) * 128, :], bnc[l][p * 32:(p + 1) * 32, :], r=["b%s%d" % (nm, l)], w=["%s%d" % (nm, l)])
                else:
                    P.allgather(dst_[l], bnc[l], r=["b%s%d" % (nm, l)], w=["%s%d" % (nm, l)])

        load_shared(0)
        P.barrier()

        ring_i = [0]

        def ag_pieces(out_flat, in_flat, r0, npieces, r, w):
            for p in range(npieces):
                P.allgather(out_flat[p * 4 * r0:(p + 1) * 4 * r0, :], in_flat[p * r0:(p + 1) * r0, :], r=r, w=w)

        def ring_load(src_ap, key):
            i = ring_i[0] % 2
            ring_i[0] += 1
            t = wring[i][:].rearrange("p (k n) -> p k n", k=8)
            P.dma("pool", t, src_ap, r=[key] if key else [], w=["ring%d" % i])
            return t, "ring%d" % i

        def recip(eng, out, in_, r, w):
            return P.op(eng, lambda e: e.reciprocal(out, in_), r, w)

        def rms_rstd(ss, n, keys):
            P.act(ss, ss, AF.Sqrt, scale=1.0 / n, bias=epsb[:, 0:1], r=keys + ["epsb"], w=keys)
            recip("dve", ss, ss, keys, keys)

        for l in range(L):
            last = (l == L - 1)
            xdst = out if (last and final) else XC
            cv = Carver()
            cbc = cv.f32(8 * 128, ("p (k m) -> p k m", dict(k=8)))
            bada = cv.f32(6 * D)
            gtmp = cv.f32(4 * D, ("p (a n) -> p a n", dict(a=4)))
            wf = [cv.f32(8 * 512, ("p (k n) -> p k n", dict(k=8))) for _ in range(2)]
            for k in range(8):
                P.ts("dve", cbc[:, k, :], ones_f[:], cact[:, k:k + 1], ALU.mult, r=["ones_f", "cact"], w=["cbc"])
            P.dma("sp", bada, b_ada[l].partition_broadcast(128), w=["bada"])
            for a, gv in enumerate((g_pre_mix, g_post_mix, g_pre_ffn, g_post_ffn)):
                P.dma("sp", gtmp[:, a, :], gv[l].partition_broadcast(128), w=["gtmp%d" % a])
            wada_v = W_ADA[l].rearrange("(k p) n -> p k n", p=128)
            for cb in range(12):
                P.dma("sp", wf[cb % 2], wada_v[:, :, cb * 512:(cb + 1) * 512], r=["W_ADA%d" % l], w=["wf%d" % (cb % 2)])
                for k in range(8):
                    P.mm(psb(0, cb % 2), cbc[:, k, :], wf[cb % 2][:, k, :], start=(k == 0), stop=(k == 7),
                         r=["cbc", "wf%d" % (cb % 2)], w=["ps0_%d" % (cb % 2)])
                slot, half = cb // 2, cb % 2
                P.tt("dve", vec[:, slot, half * 512:(half + 1) * 512], psb(0, cb % 2), bada[:, cb * 512:(cb + 1) * 512],
                     ALU.add, r=["ps0_%d" % (cb % 2), "bada"], w=["vec%d" % slot])
            P.stt("dve", vec[:, 1, :], vec[:, 1, :], 1.0, gtmp[:, 0, :], ALU.add, ALU.mult, r=["vec1", "gtmp0"], w=["vec1"])
            P.tt("dve", vec[:, 2, :], vec[:, 2, :], gtmp[:, 1, :], ALU.mult, r=["vec2", "gtmp1"], w=["vec2"])
            P.stt("dve", vec[:, 4, :], vec[:, 4, :], 1.0, gtmp[:, 2, :], ALU.add, ALU.mult, r=["vec4", "gtmp2"], w=["vec4"])
            P.tt("dve", vec[:, 5, :], vec[:, 5, :], gtmp[:, 3, :], ALU.mult, r=["vec5", "gtmp3"], w=["vec5"])
            P.dma("sp", lamt[:].rearrange("p a n -> p (a n)"), lamv[l].rearrange("a n -> (a n)").partition_broadcast(128), w=["lamt"])
            P.tt("dve", lamt[:, 0, :], lamt[:, 0, :], lamt[:, 1, :], ALU.mult, r=["lamt"], w=["lamt"])
            P.tt("dve", lamt[:, 2, :], lamt[:, 2, :], lamt[:, 3, :], ALU.mult, r=["lamt"], w=["lamt"])
            P.red("dve", lamr[:, 0:1], lamt[:, 0, :], ALU.add, r=["lamt"], w=["lamr"])
            P.red("dve", lamr[:, 1:2], lamt[:, 2, :], ALU.add, r=["lamt"], w=["lamr"])
            P.act(lamr[:, 0:2], lamr[:, 0:2], AF.Exp, r=["lamr"], w=["lamr"])
            P.tt("dve", lamr[:, 2:3], lamr[:, 1:2], lamr[:, 0:1], ALU.subtract, r=["lamr"], w=["lamr"])
            P.tt("dve", nlam[:], lamr[:, 2:3], lamc_s[:, 2 * l:2 * l + 1], ALU.subtract, r=["lamr", "lamc"], w=["nlam"])
            P.dma("sp", gsc[:], g_subln[l].rearrange("(p o) -> p o", o=1), w=["gsc"])
            P.ts("dve", gsc[:], gsc[:], lamc_s[:, 2 * l + 1:2 * l + 2], ALU.mult, r=["gsc", "lamc"], w=["gsc"])
            P.dma("sp", wr_s[:], w_r[l].rearrange("(k p) e -> p k e", p=128), w=["wr_s"])
            P.dma("sp", br_s[:], b_r[l].partition_broadcast(128), w=["br_s"])
            if "vec" in dbg and l == 0:
                tv = tap("vec", [128, 6, D])
                P.dma("sp", tv, vec[:], r=["vec%d" % i for i in range(6)])
                tl = tap("nlam", [128, 1])
                P.dma("sp", tl, nlam[:], r=["nlam"])
            P.barrier()

            if "mix_a" in phases:
                cv = Carver()
                hT = cv.bf(8 * TOK, ("p (k t) -> p k t", dict(k=8)))
                uT = cv.bf(8 * TOK, ("p (k t) -> p k t", dict(k=8)))
                vgn = [cv.bf(D) for _ in range(2)]
                lnv = cv.f32(2 * D, ("p (a n) -> p a n", dict(a=2)))
                bsp = cv.f32(8 * 128)
                wst = cv.bf(8 * 128, ("p (g t) -> p g t", dict(g=8)))
                xt = [cv.f32(D) for _ in range(2)]
                t1 = [cv.f32(D) for _ in range(2)]
                hb = [cv.bf(D) for _ in range(2)]
                stg = [cv.f32(512) for _ in range(3)]
                stb = [cv.bf(TOK) for _ in range(2)]
                st16 = [cv.bf(512) for _ in range(3)]
                ss = cv.f32(8)
                P.dma("sp", lnv[:, 0, :], ln_v_g[l].partition_broadcast(128), w=["lnv"])
                P.dma("sp", lnv[:, 1, :], ln_v_b[l].partition_broadcast(128), w=["lnv"])
                P.dma("sp", bsp, b_sp[l].partition_broadcast(128), w=["bsp"])
                P.dma("pool", wst, wsT[l], w=["wst"])
                win_v = W_IN[l].rearrange("(k p) n -> p k n", p=128)
                nxt = ring_load(win_v[:, :, 0:D], "W_IN%d" % l)
                for t in range(NT):
                    b2 = t % 2
                    P.dma("sp", xt[b2], XC[t * 128:(t + 1) * 128, :], r=["XC"], w=["xt%d" % b2])
                    P.act(t1[b2], xt[b2], AF.Square, accum_out=ss[:, b2:b2 + 1], r=["xt%d" % b2], w=["t1%d" % b2, "ss%d" % b2])
                    rms_rstd(ss[:, b2:b2 + 1], D, ["ss%d" % b2])
                    P.stt("dve", t1[b2], xt[b2], ss[:, b2:b2 + 1], vec[:, 1, :], ALU.mult, ALU.mult,
                          r=["xt%d" % b2, "ss%d" % b2, "vec1"], w=["t1%d" % b2])
                    P.tt("dve", hb[b2], t1[b2], vec[:, 0, :], ALU.add, r=["t1%d" % b2, "vec0"], w=["hb%d" % b2])
                    for k in range(8):
                        P.tr(psbf(1, b2)[:, k * 128:(k + 1) * 128], hb[b2][:, k * 128:(k + 1) * 128], ident_b[:],
                             r=["hb%d" % b2, "ident_b"], w=["ps1_%d" % b2])
                    P.cp("act" if t % 2 else "dve", hT[:, :, t * 128:(t + 1) * 128],
                         psbf(1, b2).rearrange("p (k t) -> p k t", k=8), r=["ps1_%d" % b2], w=["hT"])
                if "hT" in dbg and l == 0:
                    th = tap("hT", [128, 8, TOK], BF16)
                    P.dma("sp", th, hT, r=["hT"])
                pi = [0]

                def nextps():
                    i = pi[0] % 4
                    pi[0] += 1
                    return 2 + i // 2, i % 2, "ps%d_%d" % (2 + i // 2, i % 2)

                for sec in range(7):
                    wt, wkey = nxt
                    if sec < 6:
                        nxt = ring_load(win_v[:, :, (sec + 1) * D:(sec + 2) * D], "W_IN%d" % l)
                    else:
                        nxt = ring_load(W_B[l].rearrange("(k p) n -> p k n", p=128), "W_B%d" % l)
                    if sec in (0, 1):
                        dst = QT if sec == 0 else KT_loc
                        dkey = "QT" if sec == 0 else "KT_loc"
                        for fc in range(8):
                            sbuf_i = fc % 2
                            for tg in range(4):
                                a, b_, pk = nextps()
                                for k in range(8):
                                    P.mm(psb(a, b_), wt[:, k, fc * 128:(fc + 1) * 128], hT[:, k, tg * 512:(tg + 1) * 512],
                                         start=(k == 0), stop=(k == 7), r=[wkey, "hT"], w=[pk])
                                osl = stb[sbuf_i][:, tg * 512:(tg + 1) * 512]
                                if sec == 0 and tg % 2:
                                    P.ts("dve", osl, psb(a, b_), 0.125, ALU.mult, r=[pk], w=["stb%d" % sbuf_i])
                                elif sec == 0:
                                    P.act(osl, psb(a, b_), AF.Copy, scale=0.125, r=[pk], w=["stb%d" % sbuf_i])
                                else:
                                    P.cp("dve" if tg % 2 else "act", osl, psb(a, b_), r=[pk], w=["stb%d" % sbuf_i])
                            P.dma("sp", dst[fc * 128:(fc + 1) * 128, :], stb[sbuf_i], r=["stb%d" % sbuf_i], w=[dkey])
                    elif sec == 2:
                        for tt_ in range(NT):
                            for half in range(2):
                                a, b_, pk = nextps()
                                for k in range(8):
                                    P.mm(psb(a, b_), hT[:, k, tt_ * 128:(tt_ + 1) * 128], wt[:, k, half * 512:(half + 1) * 512],
                                         start=(k == 0), stop=(k == 7), r=[wkey, "hT"], w=[pk])
                                si = (tt_ * 2 + half) % 3
                                P.cp("dve" if half else "act", st16[si], psb(a, b_), r=[pk], w=["st16%d" % si])
                                P.dma("sp", V_loc[half * 4:(half + 1) * 4, tt_ * 128:(tt_ + 1) * 128, :].rearrange("h t e -> t h e"),
                                      st16[si].rearrange("p (h e) -> p h e", h=4), r=["st16%d" % si], w=["V_loc"])
                        ag_pieces(KT_all, KT_loc, 256, 4, ["KT_loc"], ["KT_all"])
                        for hh in range(NH):
                            P.allgather(V_all[hh], V_loc[hh], r=["V_loc"], w=["V_all"])
                    elif sec == 3:
                        for fc in range(8):
                            for tg in range(4):
                                a, b_, pk = nextps()
                                for k in range(8):
                                    P.mm(psb(a, b_), wt[:, k, fc * 128:(fc + 1) * 128], hT[:, k, tg * 512:(tg + 1) * 512],
                                         start=(k == 0), stop=(k == 7), r=[wkey, "hT"], w=[pk])
                                P.act(uT[:, fc, tg * 512:(tg + 1) * 512], psb(a, b_), AF.Gelu, r=[pk], w=["uT"])
                    elif sec == 4:
                        for tt_ in range(NT):
                            b2 = tt_ % 2
                            for half in range(2):
                                for k in range(8):
                                    P.mm(ps[2 + b2][:, half, :], hT[:, k, tt_ * 128:(tt_ + 1) * 128], wt[:, k, half * 512:(half + 1) * 512],
                                         start=(k == 0), stop=(k == 7), r=[wkey, "hT"], w=["ps%d_%d" % (2 + b2, half)])
                            pv = ps[2 + b2][:].rearrange("p a n -> p (a n)")
                            P.act(xt[b2], pv, AF.Gelu, accum_out=ss[:, 2 + b2:3 + b2],
                                  r=["ps%d_0" % (2 + b2), "ps%d_1" % (2 + b2)], w=["xt%d" % b2, "ss%d" % (2 + b2)])
                            P.act(t1[b2], xt[b2], AF.Square, accum_out=ss[:, 4 + b2:5 + b2], r=["xt%d" % b2], w=["t1%d" % b2, "ss%d" % (4 + b2)])
                            m_ = ss[:, 2 + b2:3 + b2]
                            v_ = ss[:, 4 + b2:5 + b2]
                            kk = ["ss%d" % (2 + b2), "ss%d" % (4 + b2)]
                            P.ts("dve", m_, m_, 1.0 / D, ALU.mult, r=kk, w=kk)
                            P.ts("dve", v_, v_, 1.0 / D, ALU.mult, r=kk, w=kk)
                            P.tt("dve", ss[:, 6 + b2:7 + b2], m_, m_, ALU.mult, r=kk, w=["ss%d" % (6 + b2)])
                            P.tt("dve", v_, v_, ss[:, 6 + b2:7 + b2], ALU.subtract, r=kk + ["ss%d" % (6 + b2)], w=kk)
                            P.act(v_, v_, AF.Sqrt, bias=epsb[:, 0:1], r=kk + ["epsb"], w=kk)
                            recip("dve", v_, v_, kk, kk)
                            P.tt("dve", m_, m_, v_, ALU.mult, r=kk, w=kk)
                            P.ts("dve", m_, m_, -1.0, ALU.mult, r=kk, w=kk)
                            P.ts("dve", t1[b2], xt[b2], v_, ALU.mult, m_, ALU.add, r=["xt%d" % b2] + kk, w=["t1%d" % b2])
                            P.tt("dve", t1[b2], t1[b2], lnv[:, 0, :], ALU.mult, r=["t1%d" % b2, "lnv"], w=["t1%d" % b2])
                            P.tt("dve", vgn[b2], t1[b2], lnv[:, 1, :], ALU.add, r=["t1%d" % b2, "lnv"], w=["vgn%d" % b2])
                            pm = ps[b2][:].rearrange("p a n -> p (a n)")
                            pmk = ["ps%d_0" % b2, "ps%d_1" % b2]
                            for g in range(8):
                                P.mm(pm[:, g * 128:(g + 1) * 128], vgn[b2][:, g * 128:(g + 1) * 128], wst[:, g, :],
                                     r=["vgn%d" % b2, "wst"], w=pmk)
                            P.tt("dve", t1[b2], pm, bsp, ALU.add, r=pmk + ["bsp"], w=["t1%d" % b2])
                            uv = uT[:, :, tt_ * 128:(tt_ + 1) * 128]
                            P.tt("pool", uv, t1[b2].rearrange("p (g t) -> p g t", g=8), uv, ALU.mult, r=["t1%d" % b2, "uT"], w=["uT"])
                        pi[0] = 0
                    else:
                        dst = SGA if sec == 5 else SGB
                        dkey = "SGA" if sec == 5 else "SGB"
                        for fc in range(8):
                            for tg in range(4):
                                a, b_, pk = nextps()
                                for k in range(8):
                                    P.mm(psb(a, b_), wt[:, k, fc * 128:(fc + 1) * 128], hT[:, k, tg * 512:(tg + 1) * 512],
                                         start=(k == 0), stop=(k == 7), r=[wkey, "hT"], w=[pk])
                                si = (fc * 4 + tg) % 3
                                P.act(stg[si], psb(a, b_), AF.Sigmoid, r=[pk], w=["stg%d" % si])
                                P.dma("sp", dst[fc * 128:(fc + 1) * 128, tg * 512:(tg + 1) * 512], stg[si], r=["stg%d" % si], w=[dkey])
                wt, wkey = nxt
                for fc in range(8):
                    for tg in range(4):
                        a, b_, pk = nextps()
                        for k in range(8):
                            P.mm(psb(a, b_), wt[:, k, fc * 128:(fc + 1) * 128], uT[:, k, tg * 512:(tg + 1) * 512],
                                 start=(k == 0), stop=(k == 7), r=[wkey, "uT"], w=[pk])
                        si = (fc * 4 + tg) % 3
                        P.cp("dve" if tg % 2 else "act", stg[si], psb(a, b_), r=[pk], w=["stg%d" % si])
                        P.dma("sp", YB[fc * 128:(fc + 1) * 128, tg * 512:(tg + 1) * 512], stg[si], r=["stg%d" % si], w=["YB"])
                if l == 0:
                    for nm, src, shp, dt in (("QT", QT, [D, TOK], BF16), ("KT_all", KT_all, [4 * D, TOK], BF16),
                                             ("YB", YB, [D, TOK], F32),
                                             ("SGA", SGA, [D, TOK], F32)):
                        if nm in dbg:
                            tp = tap(nm, shp, dt)
                            P.dma("sp", tp, src, r=[nm])
                P.barrier()

            if "attn" in phases:
                cv = Carver()
                KTa = cv.bf(2 * SEQ, ("p (m n) -> p m n", dict(m=2)))
                KTbuf = [[KTa[:, 0, :], KTa[:, 1, :]], [wring[0][:], wring[1][:]]]
                Vbuf = [cv.bf(64 * 128, ("p (j e) -> p j e", dict(j=64))) for _ in range(2)]
                Qbuf = [cv.bf(6 * TOK, ("p (m v n) -> p m v n", dict(m=2, v=3))) for _ in range(2)]
                PT = [cv.bf(1024) for _ in range(3)]
                ppair = [cv.bf(1024) for _ in range(2)]
                accD = cv.f32(1024)
                accP = cv.f32(1024)
                rz = [cv.f32(512) for _ in range(2)]
                o0 = cv.f32(512)
                o1 = cv.f32(512)
                ob = [cv.bf(512) for _ in range(2)]
                osq = o1
                rs_ = rz[0]

                def load_head(h):
                    hb_ = h % 2
                    for m in range(2):
                        hm = m * 8 + h
                        for i in range(4):
                            P.gather(KTbuf[hb_][m][0:64, i * TOK:(i + 1) * TOK], KT_all, idxK_s[:, i * 16 + hm:i * 16 + hm + 1], 4 * D - 1,
                                     r=["KT_all", "idxK"], w=["KT%d" % hb_])
                        P.dma("pool", KTbuf[hb_][m][64:69, :], kaug[h], w=["KT%d" % hb_])
                        for v in range(3):
                            P.dma("sp", Qbuf[hb_][0:64, m, v, :], QT[hm * 64:(hm + 1) * 64, :], r=["QT"], w=["Qv%d" % hb_])
                            P.dma("pool", Qbuf[hb_][64:69, m, v, :], qaug[h, v], w=["Qv%d" % hb_])
                    for j in range(64):
                        P.gather(Vbuf[hb_][:, j, :], V_all[h], idxV_s[:, j:j + 1], SEQ - 1,
                                 r=["V_all", "idxV"], w=["Vh%d" % hb_])

                load_head(0)
                for h in range(NH):
                    if h + 1 < NH:
                        load_head(h + 1)
                    if 1 <= h <= 5 and l + 1 < L:
                        load_shared(l + 1, only=h - 1)
                    hb_ = h % 2
                    KTs = KTbuf[hb_]
                    Vh = Vbuf[hb_]
                    Qv = Qbuf[hb_]
                    kK, kV, kQ = "KT%d" % hb_, "Vh%d" % hb_, "Qv%d" % hb_
                    for g in range(4):
                        P.memset("dve", accD, 0.0, w=["accD"])
                        P.memset("dve", accP, 0.0, w=["accP"])

                        def qk(j):
                            sbuf_i = (0, 1, 3)[j % 3]
                            i, jb = j // 16, j % 16
                            for m in range(2):
                                pk = "ps%d_%d" % (sbuf_i, m)
                                lhs = KTs[m][0:69, j * 128:(j + 1) * 128]
                                if i == 0 and 4 * g <= jb <= 4 * g + 3:
                                    for qb in range(4):
                                        qs = slice(g * 512 + qb * 128, g * 512 + (qb + 1) * 128)
                                        o_ = ps[sbuf_i][:, m, qb * 128:(qb + 1) * 128]
                                        if 4 * g + qb > jb:
                                            P.mm(o_, lhs, Qv[0:69, m, 0, qs], r=[kK, kQ], w=[pk])
                                        elif 4 * g + qb < jb:
                                            P.mm(o_, lhs, Qv[0:69, m, 1, qs], r=[kK, kQ], w=[pk])
                                        else:
                                            P.mm(o_, lhs, Qv[0:69, m, 2, qs], start=True, stop=False, r=[kK, kQ], w=[pk])
                                            P.mm(o_, ident_b[:], bdg[:, h, :], start=False, stop=True, r=["ident_b", "bdg"], w=[pk])
                                else:
                                    v = 1 if (i == 0 and jb > 4 * g + 3) else 0
                                    P.mm(ps[sbuf_i][:, m, :], lhs, Qv[0:69, m, v, g * 512:(g + 1) * 512], r=[kK, kQ], w=[pk])

                        def rest(j):
                            sbuf_i = (0, 1, 3)[j % 3]
                            pb = j % 3
                            P.act(PT[pb], ps[sbuf_i][:].rearrange("p a n -> p (a n)"), AF.Exp,
                                  r=["ps%d_0" % sbuf_i, "ps%d_1" % sbuf_i], w=["PT%d" % pb])
                            if j % 2 == 1:
                                pq = (j // 2) % 2
                                P.tt("dve", ppair[pq], PT[(j - 1) % 3], PT[pb], ALU.add,
                                     r=["PT%d" % ((j - 1) % 3), "PT%d" % pb], w=["pp%d" % pq])
                                acc_, ak_ = (accD, "accD") if pq == 0 else (accP, "accP")
                                P.tt("dve", acc_, acc_, ppair[pq], ALU.add, r=["pp%d" % pq, ak_], w=[ak_])
                            for m in range(2):
                                P.mm(ps[2][:, m, :], Vh[:, j, :], PT[pb][:, m * 512:(m + 1) * 512], start=(j == 0), stop=(j == 63),
                                     r=[kV, "PT%d" % pb], w=["ps2_%d" % m])

                        qk(0)
                        qk(1)
                        for j in range(64):
                            if j + 2 < 64:
                                qk(j + 2)
                            rest(j)
                        for m in range(2):
                            P.mm(ps[3][:, m, :], ones_f[:], accD[:, m * 512:(m + 1) * 512], start=True, stop=False,
                                 r=["ones_f", "accD"], w=["ps3_%d" % m])
                            P.mm(ps[3][:, m, :], ones_f[:], accP[:, m * 512:(m + 1) * 512], start=False, stop=True,
                                 r=["ones_f", "accP"], w=["ps3_%d" % m])
                            P.op("dve", (lambda m=m: (lambda e: e.reciprocal(rz[m], ps[3][:, m, :])))(), r=["ps3_%d" % m], w=["rz%d" % m])
                        P.tt("dve", o0, ps[2][:, 0, :], rz[0], ALU.mult, r=["ps2_0", "rz0"], w=["o0"])
                        P.tt("dve", o1, ps[2][:, 1, :], rz[1], ALU.mult, r=["ps2_1", "rz1"], w=["o1"])
                        P.stt("dve", o0, o1, nlam[:, 0:1], o0, ALU.mult, ALU.add, r=["o0", "o1", "nlam"], w=["o0"])
                        P.act(osq, o0, AF.Square, r=["o0"], w=["o1"])
                        P.mm(ps[3][:, 0, :], ones_f[:], osq, r=["ones_f", "o1"], w=["ps3_0"])
                        P.act(rs_, ps[3][:, 0, :], AF.Sqrt, scale=1.0 / 128, bias=epsb[:, 0:1], r=["ps3_0", "epsb"], w=["rz0"])
                        recip("dve", rs_, rs_, ["rz0"], ["rz0"])
                        bi = (h * 4 + g) % 2
                        P.stt("dve", ob[bi], o0, gsc[:, 0:1], rs_, ALU.mult, ALU.mult, r=["o0", "gsc", "rz0"], w=["ob%d" % bi])
                        P.dma("sp", OT[h * 128:(h + 1) * 128, g * 512:(g + 1) * 512], ob[bi], r=["ob%d" % bi], w=["OT"])
                if "OT" in dbg and l == 0:
                    tp = tap("OT", [D, TOK], BF16)
                    P.dma("sp", tp, OT, r=["OT"])
                P.barrier()

            if "mix_b" in phases:
                cv = Carver()
                oTs = cv.bf(8 * TOK, ("p (k t) -> p k t", dict(k=8)))
                mT = cv.bf(8 * TOK, ("p (k t) -> p k t", dict(k=8)))
                la = [cv.f32(512) for _ in range(2)]
                lb = [cv.f32(512) for _ in range(2)]
                ly = [cv.f32(512) for _ in range(2)]
                m1 = [cv.f32(512) for _ in range(2)]
                m2 = [cv.f32(512) for _ in range(2)]
                xt = [cv.f32(D) for _ in range(2)]
                xn = [cv.f32(D) for _ in range(2)]
                t1_ = cv.f32(D)
                t1 = [t1_, t1_]
                h2 = [cv.f32(D) for _ in range(2)]
                h2T_ = cv.f32(8 * 128, ("p (k t) -> p k t", dict(k=8)))
                h2T = [h2T_, h2T_]
                lg = cv.f32(2 * NE, ("p (a e) -> p a e", dict(a=2)))
                ss = cv.f32(12)
                wa, wak = ring_load(W_A[l].rearrange("(k p) n -> p k n", p=128), "W_A%d" % l)
                wo, wok = ring_load(W_O[l].rearrange("(k p) n -> p k n", p=128), "W_O%d" % l)
                for k in range(8):
                    P.dma("sp", oTs[:, k, :], OT[k * 128:(k + 1) * 128, :], r=["OT"], w=["oTs"])
                cnt = 0
                for fc in range(8):
                    for tg in range(4):
                        b2 = cnt % 2
                        cnt += 1
                        rs = slice(fc * 128, (fc + 1) * 128)
                        cs = slice(tg * 512, (tg + 1) * 512)
                        P.dma("sp", la[b2], SGA[rs, cs], r=["SGA"], w=["la%d" % b2])
                        P.dma("sp", lb[b2], SGB[rs, cs], r=["SGB"], w=["lb%d" % b2])
                        P.dma("sp", ly[b2], YB[rs, cs], r=["YB"], w=["ly%d" % b2])
                        for k in range(8):
                            P.mm(psb(0, b2), wa[:, k, rs], oTs[:, k, cs], start=(k == 0), stop=(k == 7), r=[wak, "oTs"], w=["ps0_%d" % b2])
                        P.tt("dve", m1[b2], psb(0, b2), la[b2], ALU.mult, r=["ps0_%d" % b2, "la%d" % b2], w=["m1%d" % b2])
                        P.tt("pool", m2[b2], lb[b2], ly[b2], ALU.mult, r=["lb%d" % b2, "ly%d" % b2], w=["m2%d" % b2])
                        P.tt("dve", mT[:, fc, cs], m1[b2], m2[b2], ALU.add, r=["m1%d" % b2, "m2%d" % b2], w=["mT"])
                for t in range(NT):
                    b2 = t % 2
                    tsl = slice(t * 128, (t + 1) * 128)
                    P.dma("sp", xt[b2], XC[tsl, :], r=["XC"], w=["xt%d" % b2])
                    for half in range(2):
                        for k in range(8):
                            P.mm(ps[1][:, half, :], mT[:, k, tsl], wo[:, k, half * 512:(half + 1) * 512], start=(k == 0), stop=(k == 7),
                                 r=[wok, "mT"], w=["ps1_%d" % half])
                    pv = ps[1][:].rearrange("p a n -> p (a n)")
                    s0 = ss[:, b2:b2 + 1]
                    P.act(t1[b2], pv, AF.Square, accum_out=s0, r=["ps1_0", "ps1_1"], w=["t1c", "ssa%d" % b2])
                    rms_rstd(s0, D, ["ssa%d" % b2])
                    P.stt("dve", t1[b2], pv, s0, vec[:, 2, :], ALU.mult, ALU.mult, r=["ps1_0", "ps1_1", "ssa%d" % b2, "vec2"], w=["t1c"])
                    P.tt("dve", xn[b2], t1[b2], xt[b2], ALU.add, r=["t1c", "xt%d" % b2], w=["xn%d" % b2])
                    P.dma("sp", XC[tsl, :], xn[b2], r=["xn%d" % b2], w=["XC"])
                    s1 = ss[:, 2 + b2:3 + b2]
                    P.act(t1[b2], xn[b2], AF.Square, accum_out=s1, r=["xn%d" % b2], w=["t1c", "ssb%d" % b2])
                    rms_rstd(s1, D, ["ssb%d" % b2])
                    P.stt("dve", t1[b2], xn[b2], s1, vec[:, 4, :], ALU.mult, ALU.mult, r=["xn%d" % b2, "ssb%d" % b2, "vec4"], w=["t1c"])
                    P.tt("dve", h2[b2], t1[b2], vec[:, 3, :], ALU.add, r=["t1c", "vec3"], w=["h2%d" % b2])
                    P.dma("sp", H2_loc[tsl, :], h2[b2], r=["h2%d" % b2], w=["H2_loc%d" % (t // 2)])
                    if t % 2 == 1:
                        p_ = t // 2
                        P.allgather(H2_all[p_ * 1024:(p_ + 1) * 1024, :], H2_loc[p_ * 256:(p_ + 1) * 256, :], r=["H2_loc%d" % p_], w=["H2_all"])
                    for k in range(8):
                        P.tr(ps[2][:].rearrange("p a n -> p (a n)")[:, k * 128:(k + 1) * 128], h2[b2][:, k * 128:(k + 1) * 128], ident_f[:],
                             r=["h2%d" % b2, "ident_f"], w=["ps2_0", "ps2_1"])
                    P.cp("act", h2T[b2], ps[2][:].rearrange("p a (k t) -> p (a k) t", t=128), r=["ps2_0", "ps2_1"], w=["h2Tc"])
                    for k in range(8):
                        P.mm(ps[3][:, 0, 0:NE], h2T[b2][:, k, :], wr_s[:, k, :], start=(k == 0), stop=(k == 7),
                             r=["h2Tc", "wr_s"], w=["ps3_0"])
                    P.tt("dve", lg[:, 0, :], ps[3][:, 0, 0:NE], br_s[:], ALU.add, r=["ps3_0", "br_s"], w=["lg"])
                    s2 = ss[:, 4 + b2:5 + b2]
                    s3 = ss[:, 6 + b2:7 + b2]
                    P.red("dve", s2, lg[:, 0, :], ALU.max, r=["lg"], w=["ssc%d" % b2])
                    P.ts("dve", s2, s2, -1.0, ALU.mult, r=["ssc%d" % b2], w=["ssc%d" % b2])
                    P.act(lg[:, 1, :], lg[:, 0, :], AF.Exp, bias=s2, accum_out=s3, r=["lg", "ssc%d" % b2], w=["lg", "ssd%d" % b2])
                    P.op("dve", (lambda s3=s3: (lambda e: e.reciprocal(s3, s3)))(), r=["ssd%d" % b2], w=["ssd%d" % b2])
                    P.ts("dve", aff_own[:, t, :], lg[:, 1, :], s3, ALU.mult, r=["lg", "ssd%d" % b2], w=["aff_own"])
                P.dma("sp", AFF_loc.rearrange("(t p) e -> p t e", p=128), aff_own[:], r=["aff_own"], w=["AFF_loc"])
                P.allgather(AFF_all, AFF_loc, r=["AFF_loc"], w=["AFF_all"])
                if l == 0:
                    if "XC1" in dbg:
                        tp = tap("XC1", [TOK, D])
                        P.dma("sp", tp, XC, r=["XC"])
                    if "AFF_all" in dbg:
                        tp = tap("AFF_all", [SEQ, NE])
                        P.dma("sp", tp, AFF_all, r=["AFF_all"])
                P.barrier()

            if "moe_r" in phases:
                cv = Carver()
                affA = cv.f32(64 * NE, ("p (j e) -> p j e", dict(j=64)))
                msk = cv.f32(64 * NE, ("p (j e) -> p j e", dict(j=64)))
                lo = cv.f32(NE)
                hi = cv.f32(NE)
                mid = cv.f32(NE)
                dd = cv.f32(NE)
                ge = cv.f32(NE)
                cpart = cv.f32(NE)
                tot = cv.f32(64 * NE, ("p (j e) -> p j e", dict(j=64)))
                cs0 = cv.f32(64 * NE, ("p (j e) -> p j e", dict(j=64)))
                cs1 = cv.f32(64 * NE, ("p (j e) -> p j e", dict(j=64)))
                pos = cv.f32(64 * NE, ("p (j e) -> p j e", dict(j=64)))
                idf = cv.f32(64 * 2 * NE, ("p (j c) -> p j c", dict(j=64)))
                selt = cv.f32(EOWN * 64 * NE, ("p (i n) -> p i n", dict(i=EOWN)))
                tmp = cv.f32(64 * NE, ("p (j e) -> p j e", dict(j=64)))
                pI = cv.f32(EOWN * 64, ("p (i j) -> p i j", dict(i=EOWN)))
                mI = cv.f32(EOWN * 64, ("p (i j) -> p i j", dict(i=EOWN)))
                pIi = cv.i32(EOWN * 64).rearrange("p (i j) -> p i j", i=EOWN)
                hx = [cv.f32(D) for _ in range(8)]
                P.dma("sp", affA, AFF_all.rearrange("(j t) e -> t j e", t=128), r=["AFF_all"], w=["affA"])

                P.dma("sp", selt, selb, w=["selt"])
                P.memset("dve", lo, 0.0, w=["lo"])
                P.memset("dve", hi, 1.0, w=["hi"])
                kb = ["lo", "hi", "mid", "dd", "ge"]

                def mask_ge(thr, key):
                    for e_ in range(NE):
                        P.ts("dve", msk[:, :, e_], affA[:, :, e_], thr[:, e_:e_ + 1], ALU.is_ge, r=["affA", key], w=["msk"])

                for it in range(NBIS):
                    P.tt("dve", mid, lo, hi, ALU.add, r=kb, w=kb)
                    P.ts("dve", mid, mid, 0.5, ALU.mult, r=kb, w=kb)
                    mask_ge(mid, "mid")
                    P.red("dve", cpart, msk[:].rearrange("p j e -> p e j"), ALU.add, r=["msk"], w=["cpart"])
                    P.mm(ps[0][:, 0, 0:NE], ones_f[:], cpart, r=["ones_f", "cpart"], w=["ps0_0"])
                    P.ts("dve", ge, ps[0][:, 0, 0:NE], CAP - 0.5, ALU.is_ge, r=["ps0_0"], w=kb)
                    P.tt("dve", dd, mid, lo, ALU.subtract, r=kb, w=kb)
                    P.tt("dve", hi, hi, dd, ALU.subtract, r=kb, w=kb)
                    P.tt("dve", dd, dd, ge, ALU.mult, r=kb, w=kb)
                    P.tt("dve", lo, lo, dd, ALU.add, r=kb, w=kb)
                    P.tt("dve", hi, hi, dd, ALU.add, r=kb, w=kb)
                mask_ge(lo, "lo")
                mflat = msk[:].rearrange("p j e -> p (j e)")
                for half in range(2):
                    P.mm(ps[1][:, half, :], ut_f[:], mflat[:, half * 512:(half + 1) * 512], r=["ut_f", "msk"], w=["ps1_%d" % half])
                    P.mm(ps[2][:, half, :], ones_f[:], mflat[:, half * 512:(half + 1) * 512], r=["ones_f", "msk"], w=["ps2_%d" % half])
                P.cp("dve", tot[:].rearrange("p j e -> p (j e)"), ps[2][:].rearrange("p a n -> p (a n)"), r=["ps2_0", "ps2_1"], w=["tot"])
                P.cp("dve", cs0[:].rearrange("p j e -> p (j e)"), tot[:].rearrange("p j e -> p (j e)"), r=["tot"], w=["cs0"])
                src, dst, sk, dk = cs0, cs1, "cs0", "cs1"
                sh = 1
                while sh < 64:
                    P.cp("dve", dst[:, 0:sh, :], src[:, 0:sh, :], r=[sk], w=[dk])
                    P.tt("dve", dst[:, sh:64, :], src[:, sh:64, :], src[:, 0:64 - sh, :], ALU.add, r=[sk], w=[dk])
                    src, dst, sk, dk = dst, src, dk, sk
                    sh *= 2
                P.tt("dve", pos[:].rearrange("p j e -> p (j e)"), src[:].rearrange("p j e -> p (j e)"), tot[:].rearrange("p j e -> p (j e)"),
                     ALU.subtract, r=[sk, "tot"], w=["pos"])
                P.tt("dve", pos[:].rearrange("p j e -> p (j e)"), pos[:].rearrange("p j e -> p (j e)"), ps[1][:].rearrange("p a n -> p (a n)"),
                     ALU.add, r=["pos", "ps1_0", "ps1_1"], w=["pos"])
                P.ts("dve", tmp[:].rearrange("p j e -> p (j e)"), pos[:].rearrange("p j e -> p (j e)"), CAP - 0.5, ALU.is_le, r=["pos"], w=["tmp"])
                P.tt("dve", msk[:].rearrange("p j e -> p (j e)"), msk[:].rearrange("p j e -> p (j e)"), tmp[:].rearrange("p j e -> p (j e)"),
                     ALU.mult, r=["msk", "tmp"], w=["msk"])
                for i in range(EOWN):
                    P.tt("dve", tmp[:].rearrange("p j e -> p (j e)"), pos[:].rearrange("p j e -> p (j e)"), selt[:, i, :], ALU.mult,
                         r=["pos", "selt"], w=["tmp"])
                    P.red("dve", pI[:, i, :], tmp, ALU.add, r=["tmp"], w=["pI"])
                    P.tt("dve", tmp[:].rearrange("p j e -> p (j e)"), msk[:].rearrange("p j e -> p (j e)"), selt[:, i, :], ALU.mult,
                         r=["msk", "selt"], w=["tmp"])
                    P.red("dve", mI[:, i, :], tmp, ALU.add, r=["tmp"], w=["mI"])
                pf = pI[:].rearrange("p i j -> p (i j)")
                mf = mI[:].rearrange("p i j -> p (i j)")
                P.ts("dve", pf, pf, -BIG, ALU.add, r=["pI"], w=["pI"])
                P.tt("dve", pf, pf, mf, ALU.mult, r=["pI", "mI"], w=["pI"])
                for i in range(EOWN):
                    P.ts("dve", pI[:, i, :], pI[:, i, :], BIG + i * CAP, ALU.add, r=["pI"], w=["pI"])
                P.cp("dve", pIi[:].rearrange("p i j -> p (i j)"), pf, r=["pI"], w=["pIi"])
                pflat = pos[:].rearrange("p j e -> p (j e)")
                tflat = tmp[:].rearrange("p j e -> p (j e)")
                c0f = cs0[:].rearrange("p j e -> p (j e)")
                P.ts("dve", c0f, pflat, 255.5, ALU.is_ge, r=["pos"], w=["cs0"])
                for thr_ in (511.5, 767.5):
                    P.ts("dve", tflat, pflat, thr_, ALU.is_ge, r=["pos"], w=["tmp"])
                    P.tt("dve", c0f, c0f, tflat, ALU.add, r=["cs0", "tmp"], w=["cs0"])
                P.stt("dve", c0f, c0f, 768.0, pflat, ALU.mult, ALU.add, r=["cs0", "pos"], w=["cs0"])
                for e_ in range(NE):
                    base_e = (e_ % 4) * 4096 + (e_ // 4) * 256
                    P.ts("dve", idf[:, :, e_], cs0[:, :, e_], float(base_e) - BIG, ALU.add, r=["cs0"], w=["idf"])
                P.tt("dve", idf[:, :, 0:NE], idf[:, :, 0:NE], msk, ALU.mult, r=["idf", "msk"], w=["idf"])
                P.ts("dve", idf[:, :, 0:NE], idf[:, :, 0:NE], BIG, ALU.add, r=["idf"], w=["idf"])
                P.cp("dve", idf[:, :, NE:2 * NE], msk, r=["msk"], w=["idf"])
                P.dma("sp", IDXF.rearrange("(j t) c -> t j c", t=128), idf, r=["idf"], w=["IDXF"])
                for j in range(64):
                    hb_ = j % 8
                    tl_ = (j % 16) * 128
                    row0 = (tl_ // 256) * 1024 + (j // 16) * 256 + tl_ % 256
                    P.dma("sp", hx[hb_], H2_all[row0:row0 + 128, :], r=["H2_all"], w=["hx%d" % hb_])
                    for i in range(EOWN):
                        P.scatter(XG, pIi[:, i, j:j + 1], hx[hb_], EOWN * CAP - 1, r=["hx%d" % hb_, "pIi"], w=["XG"])
                if l == 0:
                    if "IDXF" in dbg:
                        tp = tap("IDXF", [SEQ, 2 * NE])
                        P.dma("sp", tp, IDXF, r=["IDXF"])
                    if "XG" in dbg:
                        tp = tap("XG", [EOWN * CAP, D])
                        P.dma("sp", tp, XG, r=["XG"])
                P.barrier()

            if "moe_e" in phases:
                cv = Carver()
                XT = cv.bf(8 * CAP, ("p (k s) -> p k s", dict(k=8)))
                hid = cv.bf(16 * CAP, ("p (f s) -> p f s", dict(f=16)))
                xg = [cv.bf(D) for _ in range(2)]
                sg_ = [cv.f32(512) for _ in range(2)]
                ys = [cv.f32(D) for _ in range(2)]
                wp = [cv.bf(8 * 512) for _ in range(5)]
                stg32 = [cv.f32(8 * 512) for _ in range(2)]
                wpi = [0]
                hwi = [0]

                def piece(src_ap, shape, hw=False):
                    i = wpi[0] % 5
                    wpi[0] += 1
                    t = wp[i].rearrange(shape[0], **shape[1])
                    if hw:
                        k_ = hwi[0] % 2
                        hwi[0] += 1
                        st_ = stg32[k_].rearrange(shape[0], **shape[1])
                        P.dma("sp", st_, src_ap, w=["stg32_%d" % k_])
                        P.cp("act", t, st_, r=["stg32_%d" % k_], w=["wp%d" % i])
                    else:
                        P.dma("pool", t, src_ap, w=["wp%d" % i])
                    return t, "wp%d" % i

                for i in range(n_own):
                    for sbk in range(8):
                        b2 = sbk % 2
                        P.dma("pool", xg[b2], XG[i * CAP + sbk * 128:i * CAP + (sbk + 1) * 128, :], r=["XG"], w=["xg%d" % b2])
                        for k in range(8):
                            P.tr(psbf(3, b2)[:, k * 128:(k + 1) * 128], xg[b2][:, k * 128:(k + 1) * 128], ident_b[:],
                                 r=["xg%d" % b2, "ident_b"], w=["ps3_%d" % b2])
                        P.cp("dve", XT[:, :, sbk * 128:(sbk + 1) * 128], psbf(3, b2).rearrange("p (k t) -> p k t", k=8),
                             r=["ps3_%d" % b2], w=["XT"])
                    wgv = w_ge[l, i].rearrange("(k p) f -> p k f", p=128)
                    wuv = w_ue[l, i].rearrange("(k p) f -> p k f", p=128)
                    wdv = w_de[l, i].rearrange("(f p) n -> p f n", p=128)
                    cnt = 0
                    for fb in range(4):
                        wg_, wgk = piece(wgv[:, :, fb * 512:(fb + 1) * 512], ("p (k n) -> p k n", dict(k=8)))
                        wu_, wuk = piece(wuv[:, :, fb * 512:(fb + 1) * 512], ("p (k n) -> p k n", dict(k=8)), hw=True)
                        for fc in range(4):
                            for sgi in range(2):
                                b2 = cnt % 2
                                cnt += 1
                                ssl = slice(sgi * 512, (sgi + 1) * 512)
                                for k in range(8):
                                    P.mm(ps[b2][:, 0, :], wg_[:, k, fc * 128:(fc + 1) * 128], XT[:, k, ssl], start=(k == 0), stop=(k == 7),
                                         r=[wgk, "XT"], w=["ps%d_0" % b2])
                                for k in range(8):
                                    P.mm(ps[b2][:, 1, :], wu_[:, k, fc * 128:(fc + 1) * 128], XT[:, k, ssl], start=(k == 0), stop=(k == 7),
                                         r=[wuk, "XT"], w=["ps%d_1" % b2])
                                P.act(sg_[b2], ps[b2][:, 0, :], AF.Silu, r=["ps%d_0" % b2], w=["sg%d" % b2])
                                P.tt("dve", hid[:, fb * 4 + fc, ssl], sg_[b2], ps[b2][:, 1, :], ALU.mult, r=["sg%d" % b2, "ps%d_1" % b2], w=["hid"])
                    wds = [piece(wdv[:, q * 4:(q + 1) * 4, :], ("p (f n) -> p f n", dict(f=4)), hw=(q % 2 == 1)) for q in range(4)]
                    for sbk in range(8):
                        b2 = sbk % 2
                        for half in range(2):
                            for f in range(16):
                                wd_, wdk = wds[f // 4]
                                P.mm(ps[2][:, half, :], hid[:, f, sbk * 128:(sbk + 1) * 128], wd_[:, f % 4, half * 512:(half + 1) * 512],
                                     start=(f == 0), stop=(f == 15), r=["hid", wdk], w=["ps2_%d" % half])
                        P.cp("act", ys[b2], ps[2][:].rearrange("p a n -> p (a n)"), r=["ps2_0", "ps2_1"], w=["ys%d" % b2])
                        P.dma("sp", Y_loc[i * CAP + sbk * 128:i * CAP + (sbk + 1) * 128, :], ys[b2], r=["ys%d" % b2], w=["Y_loc%d" % i])
                    for p in range(4 * i, 4 * i + 4):
                        P.allgather(Y_all[p * 1024:(p + 1) * 1024, :], Y_loc[p * 256:(p + 1) * 256, :], r=["Y_loc%d" % i], w=["Y_all"])
                if "Y_loc" in dbg and l == 0:
                    tp = tap("Y_loc", [EOWN * CAP, D])
                    P.dma("sp", tp, Y_loc, r=["Y_loc%d" % i for i in range(n_own)])
                P.barrier()

            if "moe_c" in phases:
                cv = Carver()
                own = cv.f32(NT * 2 * NE, ("p (t c) -> p t c", dict(t=NT)))
                gidx = cv.i32(NT * NE).rearrange("p (t e) -> p t e", t=NT)
                gate = cv.f32(NT * NE, ("p (t e) -> p t e", dict(t=NT)))
                G = [cv.f32(D) for _ in range(4)]
                accA = cv.f32(D)
                accB = cv.f32(D)
                xt = [cv.f32(D) for _ in range(2)]
                t1 = cv.f32(D)
                ss = cv.f32(4)
                for q in range(4):
                    P.memset("dve", G[q], 0.0, w=["G%d" % q])
                for t in range(NT):
                    P.gather(own[:, t, :], IDXF, idxO_s[:, t:t + 1], SEQ - 1, r=["IDXF", "idxO"], w=["own"])
                P.cp("dve", gidx, own[:, :, 0:NE], r=["own"], w=["gidx"])
                P.tt("dve", gate, own[:, :, NE:2 * NE], aff_own[:], ALU.mult, r=["own", "aff_own"], w=["gate"])
                for t in range(NT):
                    b2 = t % 2
                    tsl = slice(t * 128, (t + 1) * 128)
                    P.dma("sp", xt[b2], XC[tsl, :], r=["XC"], w=["xt%d" % b2])
                    for e_ in range(NE):
                        q = e_ % 4
                        P.gather(G[q], Y_all, gidx[:, t, e_:e_ + 1], NE * CAP - 1, r=["Y_all", "gidx"], w=["G%d" % q])
                        eng, acc, ak = ("dve", accA, "accA") if e_ % 2 == 0 else ("dve", accB, "accB")
                        if e_ < 2:
                            P.ts(eng, acc, G[q], gate[:, t, e_:e_ + 1], ALU.mult, r=["G%d" % q, "gate"], w=[ak])
                        elif eng == "dve":
                            P.stt(eng, acc, G[q], gate[:, t, e_:e_ + 1], acc, ALU.mult, ALU.add, r=["G%d" % q, "gate", ak], w=[ak])
                        else:
                            P.ts(eng, G[q], G[q], gate[:, t, e_:e_ + 1], ALU.mult, r=["G%d" % q, "gate"], w=["G%d" % q])
                            P.tt(eng, acc, acc, G[q], ALU.add, r=["G%d" % q, ak], w=[ak])
                    P.tt("dve", accA, accA, accB, ALU.add, r=["accA", "accB"], w=["accA"])
                    s0 = ss[:, b2:b2 + 1]
                    P.act(t1, accA, AF.Square, accum_out=s0, r=["accA"], w=["t1", "ss%d" % b2])
                    rms_rstd(s0, D, ["ss%d" % b2])
                    P.stt("dve", t1, accA, s0, vec[:, 5, :], ALU.mult, ALU.mult, r=["accA", "ss%d" % b2, "vec5"], w=["t1"])
                    P.tt("dve", xt[b2], t1, xt[b2], ALU.add, r=["t1", "xt%d" % b2], w=["xt%d" % b2])
                    P.dma("sp", xdst[tsl, :], xt[b2], r=["xt%d" % b2], w=["XC" if xdst is XC else "out"])
                P.barrier()
            elif last and final:
                P.dma("sp", out, XC, r=["XC"], w=["out"])
                P.barrier()

        with nc.Block() as block:
            P.emit(block)
    return nc, taps


def _const_tables(r):
    slopes = [2.0 ** (-(h + 1)) for h in range(NH)]
    ql = np.arange(TOK)
    q_hi = (ql // 64) * 64
    q_lo = ql % 64
    qaug = np.zeros((NH, 3, 5, TOK), np.float32)
    kaug = np.zeros((NH, 5, SEQ), np.float32)
    bdiag = np.zeros((128, NH, 128), np.float32)
    kl = np.arange(TOK)
    k_hi = (kl // 64) * 64
    k_lo = kl % 64
    dloc = np.abs(np.arange(128)[:, None] - np.arange(128)[None, :]).astype(np.float32)
    for h, c in enumerate(slopes):
        below = np.stack([c * q_hi, c * q_lo, np.ones(TOK), np.ones(TOK), np.ones(TOK)]).astype(np.float32)
        qaug[h, 0] = below
        qaug[h, 1] = -below
        qaug[h, 2] = 0.0
        for i in range(4):
            sl = slice(i * TOK, (i + 1) * TOK)
            if i == 0:
                sg, dl = 1.0, 0.0
            else:
                pr = (r + i) % 4
                delta = 2048.0 * (r - pr)
                sg = 1.0 if delta > 0 else -1.0
                dl = abs(delta)
            kaug[h, 0, sl] = -sg
            kaug[h, 1, sl] = -sg
            kaug[h, 2, sl] = sg * c * k_hi
            kaug[h, 3, sl] = sg * c * k_lo
            kaug[h, 4, sl] = -c * dl
        bdiag[:, h, :] = -c * dloc
    idxK = np.zeros((64, 64), np.int32)
    for i in range(4):
        for hm in range(16):
            idxK[:, i * 16 + hm] = (hm // 4) * 1024 + ((r + i) % 4) * 256 + (hm % 4) * 64 + np.arange(64)
    idxV = np.zeros((128, 64), np.int32)
    for j in range(64):
        idxV[:, j] = ((r + j // 16) % 4) * TOK + (j % 16) * 128 + np.arange(128)
    idxOwn = np.zeros((128, NT), np.int32)
    for t in range(NT):
        idxOwn[:, t] = r * TOK + t * 128 + np.arange(128)
    selb = np.zeros((128, EOWN, 64, NE), np.float32)
    for i in range(EOWN):
        selb[:, i, :, 4 * r + i] = 1.0
    return dict(qaug=qaug, kaug=kaug, bdiag=bdiag, idxK=idxK, idxV=idxV, idxOwn=idxOwn,
                selb=selb.reshape(128, EOWN, 64 * NE))


def make_in_maps(inp, layers, xs=None, n_own=EOWN, with_experts=True):
    L = len(layers)
    ls = list(layers)
    f = lambda a: np.ascontiguousarray(a, dtype=np.float32)
    ident = np.eye(128, dtype=np.float32)
    utri = np.triu(np.ones((128, 128), np.float32), k=1)
    lamc = np.zeros((128, 2 * L), np.float32)
    for i, l in enumerate(ls):
        li = 0.8 - 0.6 * math.exp(-0.3 * l)
        lamc[:, 2 * i] = li
        lamc[:, 2 * i + 1] = 1.0 - li
    lamv = np.stack([np.stack([inp["lam_q1"][l], inp["lam_k1"][l], inp["lam_q2"][l], inp["lam_k2"][l]]) for l in ls])
    wsT = np.stack([np.transpose(inp["w_spatial"][l], (2, 0, 1)) for l in ls])
    b_sp = np.stack([inp["b_spatial"][l].reshape(-1) for l in ls])
    maps = []
    for c in range(8):
        b, r = c // 4, c % 4
        rs = slice(r * 256, (r + 1) * 256)
        rp = (np.arange(8)[:, None] * 128 + r * 32 + np.arange(32)[None, :]).reshape(-1)
        m = dict(
            x=f(inp["x"][b, r * TOK:(r + 1) * TOK] if xs is None else xs[c]),
            c_pk=f(np.asarray(inp["c"][b]).reshape(8, 128).T),
            w_ada_s=f(np.stack([inp["w_ada"][l][rp] for l in ls])),
            w_in_s=f(np.stack([inp["w_in"][l][rp] for l in ls])),
            w_a_s=f(np.stack([inp["w_branch_a"][l][rs] for l in ls])),
            w_b_s=f(np.stack([inp["w_branch_b"][l][rs] for l in ls])),
            w_o_s=f(np.stack([inp["w_out"][l][rs] for l in ls])),
            b_ada=f(np.stack([inp["b_ada"][l] for l in ls])),
            g_pre_mix=f(np.stack([inp["g_pre_mix"][l] for l in ls])),
            g_post_mix=f(np.stack([inp["g_post_mix"][l] for l in ls])),
            g_pre_ffn=f(np.stack([inp["g_pre_ffn"][l] for l in ls])),
            g_post_ffn=f(np.stack([inp["g_post_ffn"][l] for l in ls])),
            ln_v_g=f(np.stack([inp["ln_v_g"][l] for l in ls])),
            ln_v_b=f(np.stack([inp["ln_v_b"][l] for l in ls])),
            g_subln=f(np.stack([inp["g_subln"][l] for l in ls])),
            lamv=f(lamv), lamc=lamc, wsT=f(wsT), b_sp=f(b_sp),
            w_r=f(np.stack([inp["w_router"][l] for l in ls])),
            b_r=f(np.stack([inp["b_router"][l] for l in ls])),
            ident=ident, utri=utri,
        )
        if with_experts:
            m["w_ge"] = f(np.stack([inp["w_gate_e"][l][4 * r:4 * r + n_own] for l in ls]))
            m["w_ue"] = f(np.stack([inp["w_up_e"][l][4 * r:4 * r + n_own] for l in ls]))
            m["w_de"] = f(np.stack([inp["w_down_e"][l][4 * r:4 * r + n_own] for l in ls]))
        m.update(_const_tables(r))
        maps.append(m)
    return maps


_PROG_CACHE = {}
FUSED = True


def _get_prog(L):
    if L not in _PROG_CACHE:
        lam_inits = [0.8 - 0.6 * math.exp(-0.3 * l) for l in range(L)]
        _PROG_CACHE[L] = build_program(L, lam_inits)[0]
    return _PROG_CACHE[L]


def kernel(**inputs):
    inp = {k: np.asarray(v) for k, v in inputs.items()}
    depth = 4
    if FUSED:
        nc = _get_prog(depth)
        maps = make_in_maps(inp, range(depth))
        res = run_bass_kernel_spmd(nc, maps, core_ids=list(range(8)))
        shards = [res.results[c]["out"] for c in range(8)]
    else:
        nc = _get_prog(1)
        shards = None
        for l in range(depth):
            maps = make_in_maps(inp, [l], xs=shards)
            res = run_bass_kernel_spmd(nc, maps, core_ids=list(range(8)))
            shards = [np.asarray(res.results[c]["out"]) for c in range(8)]
    outp = np.zeros((2, SEQ, D), np.float32)
    for c in range(8):
        b, r = c // 4, c % 4
        outp[b, r * TOK:(r + 1) * TOK] = shards[c]
    return outp
```

```python
import math
from contextlib import ExitStack

import numpy as np
import concourse.bass as bass
import concourse.mybir as mybir
from concourse.bass_utils import run_bass_kernel_spmd

F32 = mybir.dt.float32
BF16 = mybir.dt.bfloat16
I32 = mybir.dt.int32
AF = mybir.ActivationFunctionType
ALU = mybir.AluOpType
AX = mybir.AxisListType

D = 1024
SEQ = 8192
TOK = 2048
NT = 16
NH = 8
NE = 16
EOWN = 4
CAP = 1024
FF = 2048
EPS = 1e-6
NBIS = 26
BIG = 1.0e6


class Op:
    __slots__ = ("eng", "fn", "kind", "inc", "seq", "sem", "val", "count")

    def __init__(self, eng, fn, kind):
        self.eng = eng
        self.fn = fn
        self.kind = kind
        self.inc = False
        self.seq = -1
        self.sem = None
        self.val = 0
        self.count = 0


ENGS = ("pe", "act", "dve", "pool", "sp")


class Prog:
    def __init__(self, nc, es):
        self.nc = nc
        self.items = {e: [] for e in ENGS}
        self.nseq = {e: 0 for e in ENGS}
        self.lastw = {}
        self.rd_c = {}
        self.rd_d = {}
        self.waited = {e: {} for e in ENGS}
        self.waited_d = {e: set() for e in ENGS}
        self.last_c = {e: None for e in ENGS}
        self.pending_d = []
        self.esem = {e: es.enter_context(nc.semaphore("s_" + e)) for e in ENGS}
        ndma = {"sp": 24, "pool": 40, "act": 2}
        self.dsem = {e: [es.enter_context(nc.semaphore("d_%s%d" % (e, i))) for i in range(n)]
                     for e, n in ndma.items()}
        self.dsem_use = {e: [0] * n for e, n in ndma.items()}
        self.dsem_last = {e: [None] * n for e, n in ndma.items()}
        self.dsem_next = {e: 0 for e in ndma}
        self.csem = es.enter_context(nc.semaphore("cc"))
        self.csem_use = 0

    def _need(self, eng, d):
        if d.kind != "c":
            if id(d) in self.waited_d[eng]:
                return
            self.waited_d[eng].add(id(d))
        else:
            if d.eng == eng and eng == "pe":
                return
            if self.waited[eng].get(d.eng, -1) >= d.seq:
                return
            self.waited[eng][d.eng] = d.seq
            d.inc = True
        self.items[eng].append(("w", d))

    def op(self, eng, fn, r=(), w=(), kind="c"):
        o = Op(eng, fn, kind)
        for k in r:
            lw = self.lastw.get(k)
            if lw is not None:
                self._need(eng, lw)
        for k in w:
            lw = self.lastw.get(k)
            if lw is not None:
                self._need(eng, lw)
            for ro in self.rd_c.get(k, {}).values():
                self._need(eng, ro)
            for ro in self.rd_d.get(k, ()):
                self._need(eng, ro)
        if kind == "d":
            i = self.dsem_next[eng]
            self.dsem_next[eng] = (i + 1) % len(self.dsem[eng])
            prev = self.dsem_last[eng][i]
            if prev is not None:
                self._need(eng, prev)
            self.dsem_use[eng][i] += 1
            o.sem = self.dsem[eng][i]
            o.val = 16 * self.dsem_use[eng][i]
            self.dsem_last[eng][i] = o
            self.pending_d.append(o)
        elif kind == "x":
            self.csem_use += 1
            o.sem = self.csem
            o.val = self.csem_use
            self.pending_d.append(o)
        else:
            o.seq = self.nseq[eng]
            self.nseq[eng] += 1
            self.last_c[eng] = o
        self.items[eng].append(o)
        for k in r:
            if kind == "c":
                self.rd_c.setdefault(k, {})[eng] = o
            else:
                self.rd_d.setdefault(k, []).append(o)
        for k in w:
            self.lastw[k] = o
            self.rd_c[k] = {}
            self.rd_d[k] = []
        return o

    def barrier(self):
        latest = {}
        for d in self.pending_d:
            latest[id(d.sem)] = d
        for d in latest.values():
            self._need("sp", d)
        for e2 in ENGS:
            if e2 != "sp" and self.last_c[e2] is not None:
                self._need("sp", self.last_c[e2])
        b = Op("sp", lambda e: e.nop(), "c")
        b.seq = self.nseq["sp"]
        self.nseq["sp"] += 1
        self.last_c["sp"] = b
        self.items["sp"].append(b)
        for e in ENGS:
            if e != "sp":
                self._need(e, b)
        self.pending_d = []
        self.lastw = {}
        self.rd_c = {}
        self.rd_d = {}

    def emit(self, block):
        for e in ENGS:
            c = 0
            for it in self.items[e]:
                if isinstance(it, Op) and it.kind == "c":
                    if it.inc:
                        c += 1
                    it.count = c
        esem = self.esem

        def run(e_name):
            def body(e):
                for it in self.items[e_name]:
                    if isinstance(it, Op):
                        ins = it.fn(e)
                        if it.kind == "d":
                            ins.then_inc(it.sem, 16)
                        elif it.kind == "x":
                            ins.then_inc(it.sem, 1)
                        elif it.inc:
                            ins.then_inc(esem[e_name], 1)
                    else:
                        d = it[1]
                        if d.kind == "c":
                            e.wait_ge(esem[d.eng], d.count)
                        else:
                            e.wait_ge(d.sem, d.val)
            return body

        block.tensor(run("pe"))
        block.scalar(run("act"))
        block.vector(run("dve"))
        block.gpsimd(run("pool"))
        block.sync(run("sp"))

    def mm(self, out, lhsT, rhs, start=True, stop=True, r=(), w=()):
        return self.op("pe", lambda e: e.matmul(out, lhsT=lhsT, rhs=rhs, start=start, stop=stop), r, w)

    def tr(self, out, in_, ident, r=(), w=()):
        return self.op("pe", lambda e: e.transpose(out, in_, ident), r, w)

    def act(self, out, in_, func, r=(), w=(), bias=None, scale=None, accum_out=None):
        kw = {}
        if bias is not None:
            kw["bias"] = bias
        if scale is not None:
            kw["scale"] = scale
        if accum_out is not None:
            kw["accum_out"] = accum_out
        return self.op("act", lambda e: e.activation(out=out, in_=in_, func=func, **kw), r, w)

    def tt(self, eng, out, in0, in1, op, r=(), w=()):
        return self.op(eng, lambda e: e.tensor_tensor(out=out, in0=in0, in1=in1, op=op), r, w)

    def ts(self, eng, out, in0, s1, op0, s2=None, op1=None, r=(), w=()):
        if op1 is None:
            return self.op(eng, lambda e: e.tensor_scalar(out=out, in0=in0, scalar1=s1, scalar2=None, op0=op0), r, w)
        return self.op(eng, lambda e: e.tensor_scalar(out=out, in0=in0, scalar1=s1, scalar2=s2, op0=op0, op1=op1), r, w)

    def stt(self, eng, out, in0, scalar, in1, op0, op1, r=(), w=()):
        return self.op(eng, lambda e: e.scalar_tensor_tensor(out=out, in0=in0, scalar=scalar, in1=in1, op0=op0, op1=op1), r, w)

    def cp(self, eng, out, in_, r=(), w=()):
        if eng == "act":
            return self.op(eng, lambda e: e.copy(out=out, in_=in_), r, w)
        return self.op(eng, lambda e: e.tensor_copy(out, in_), r, w)

    def red(self, eng, out, in_, op, r=(), w=()):
        return self.op(eng, lambda e: e.tensor_reduce(out=out, in_=in_, axis=AX.X, op=op), r, w)

    def memset(self, eng, ap, val, r=(), w=()):
        return self.op(eng, lambda e: e.memset(ap, val), r, w)

    def dma(self, eng, out, in_, r=(), w=()):
        return self.op(eng, lambda e: e.dma_start(out=out, in_=in_), r, w, kind="d")

    def _breg(self, e, bound):
        if not hasattr(self, "bregs"):
            self.bregs = {}
        if bound not in self.bregs:
            rg = e.alloc_register(name="bnd%d" % bound)
            e.reg_mov(rg, bound)
            self.bregs[bound] = rg
        return self.bregs[bound]

    def gather(self, out, src, idx, bound, r=(), w=()):
        return self.op("pool", lambda e: e.indirect_dma_start(
            out=out, out_offset=None, in_=src,
            in_offset=bass.IndirectOffsetOnAxis(ap=idx, axis=0),
            bounds_check=self._breg(e, bound), oob_is_err=False), r, w, kind="d")

    def scatter(self, dst, idx, in_, bound, r=(), w=()):
        return self.op("pool", lambda e: e.indirect_dma_start(
            out=dst, out_offset=bass.IndirectOffsetOnAxis(ap=idx, axis=0),
            in_=in_, in_offset=None,
            bounds_check=self._breg(e, bound), oob_is_err=False), r, w, kind="d")

    def allgather(self, out, in_, r=(), w=()):
        return self.op("pool", lambda e: e.collective_compute(
            "AllGather", ALU.bypass, replica_groups=[[0, 1, 2, 3], [4, 5, 6, 7]],
            ins=[in_], outs=[out]), r, w, kind="x")


def build_program(L, lam_inits, dbg=None, phases=None, n_own=EOWN, final=True):
    dbg = dbg or set()
    allph = {"mix_a", "attn", "mix_b", "moe_r", "moe_e", "moe_c"}
    phases = allph if phases is None else phases
    nc = bass.Bass("TRN2", target_bir_lowering=False)

    def din(name, shape, dt=F32):
        return nc.dram_tensor(name, list(shape), dt, kind="ExternalInput").ap()

    def dint(name, shape, dt=F32):
        return nc.dram_tensor(name, list(shape), dt, kind="Internal").ap()

    def dout(name, shape, dt=F32):
        return nc.dram_tensor(name, list(shape), dt, kind="ExternalOutput").ap()

    x_in = din("x", [TOK, D])
    c_pk = din("c_pk", [128, 8])
    w_ada_s = din("w_ada_s", [L, 256, 6 * D])
    w_in_s = din("w_in_s", [L, 256, 7 * D])
    w_a_s = din("w_a_s", [L, 256, D])
    w_b_s = din("w_b_s", [L, 256, D])
    w_o_s = din("w_o_s", [L, 256, D])
    b_ada = din("b_ada", [L, 6 * D])
    g_pre_mix = din("g_pre_mix", [L, D])
    g_post_mix = din("g_post_mix", [L, D])
    g_pre_ffn = din("g_pre_ffn", [L, D])
    g_post_ffn = din("g_post_ffn", [L, D])
    ln_v_g = din("ln_v_g", [L, D])
    ln_v_b = din("ln_v_b", [L, D])
    g_subln = din("g_subln", [L, 128])
    lamv = din("lamv", [L, 4, 64])
    lamc = din("lamc", [128, 2 * L])
    wsT = din("wsT", [L, 128, 8, 128])
    b_sp = din("b_sp", [L, 8 * 128])
    w_r = din("w_r", [L, D, NE])
    b_r = din("b_r", [L, NE])
    if "moe_e" in phases:
        w_ge = din("w_ge", [L, n_own, D, FF])
        w_ue = din("w_ue", [L, n_own, D, FF])
        w_de = din("w_de", [L, n_own, FF, D])
    ident = din("ident", [128, 128])
    utri = din("utri", [128, 128])
    qaug = din("qaug", [NH, 3, 5, TOK])
    kaug = din("kaug", [NH, 5, SEQ])
    bdiag = din("bdiag", [128, NH, 128])
    idxK = din("idxK", [64, 64], I32)
    idxV = din("idxV", [128, 64], I32)
    idxOwn = din("idxOwn", [128, NT], I32)
    selb = din("selb", [128, EOWN, 64 * NE])

    out = dout("out", [TOK, D])
    taps = {}

    def tap(name, shape, dt=F32):
        if name in dbg:
            taps[name] = dout("t_" + name, shape, dt)
            return taps[name]
        return None

    W_ADA = dint("W_ADA", [L, D, 6 * D])
    W_IN = dint("W_IN", [L, D, 7 * D])
    W_A = dint("W_A", [L, D, D])
    W_B = dint("W_B", [L, D, D])
    W_O = dint("W_O", [L, D, D])
    B_ADA = dint("B_ADA", [L, 256, 6 * D])
    B_IN = dint("B_IN", [L, 256, 7 * D])
    B_A = dint("B_A", [L, 256, D])
    B_B = dint("B_B", [L, 256, D])
    B_O = dint("B_O", [L, 256, D])
    XC = dint("XC", [TOK, D])
    QT = dint("QT", [D, TOK], BF16)
    KT_loc = dint("KT_loc", [D, TOK], BF16)
    KT_all = dint("KT_all", [4 * D, TOK], BF16)
    V_loc = dint("V_loc", [NH, TOK, 128], BF16)
    V_all = [dint("V_all%d" % h, [SEQ, 128], BF16) for h in range(NH)]
    SGA = dint("SGA", [D, TOK])
    SGB = dint("SGB", [D, TOK])
    YB = dint("YB", [D, TOK])
    OT = dint("OT", [D, TOK], BF16)
    H2_loc = dint("H2_loc", [TOK, D])
    H2_all = dint("H2_all", [SEQ, D])
    AFF_loc = dint("AFF_loc", [TOK, NE])
    AFF_all = dint("AFF_all", [SEQ, NE])
    XG = dint("XG", [EOWN * CAP, D])
    Y_loc = dint("Y_loc", [EOWN * CAP, D])
    Y_all = dint("Y_all", [NE * CAP, D])
    IDXF = dint("IDXF", [SEQ, 2 * NE])

    es = ExitStack()
    with es:
        P = Prog(nc, es)

        def sb(name, shape, dt=F32):
            return es.enter_context(nc.sbuf_tensor(name, list(shape), dt))

        ident_f = sb("ident_f", [128, 128])
        ident_b = sb("ident_b", [128, 128], BF16)
        ut_f = sb("ut_f", [128, 128])
        ones_f = sb("ones_f", [128, 128])
        epsb = sb("epsb", [128, 1])
        bdg = sb("bdg", [128, NH, 128], BF16)
        idxK_s = sb("idxK_s", [64, 64], I32)
        idxV_s = sb("idxV_s", [128, 64], I32)
        idxO_s = sb("idxO_s", [128, NT], I32)
        lamc_s = sb("lamc_s", [128, 2 * L])
        cact = sb("cact", [128, 8])
        vec = sb("vec", [128, 6, D])
        gsc = sb("gsc", [128, 1])
        nlam = sb("nlam", [128, 1])
        lamt = sb("lamt", [128, 4, 64])
        lamr = sb("lamr", [128, 4])
        aff_own = sb("aff_own", [128, NT, NE])
        wr_s = sb("wr_s", [128, 8, NE])
        br_s = sb("br_s", [128, NE])
        wring = [sb("wring%d" % i, [128, 8 * D], BF16) for i in range(2)]
        ARENA = 35840
        arena = sb("arena", [128, ARENA])
        ps = [es.enter_context(nc.psum_tensor("ps%d" % i, [128, 2, 512], F32)) for i in range(4)]

        class Carver:
            def __init__(self):
                self.off = 0

            def f32(self, n, shape=None):
                a = arena[:, self.off:self.off + n]
                self.off += n
                assert self.off <= ARENA, self.off
                if shape is not None:
                    a = a.rearrange(shape[0], **shape[1])
                return a

            def bf(self, n, shape=None):
                w = (n + 1) // 2
                a = arena[:, self.off:self.off + w].bitcast(BF16)
                self.off += w
                assert self.off <= ARENA, self.off
                if shape is not None:
                    a = a.rearrange(shape[0], **shape[1])
                return a

            def i32(self, n):
                a = arena[:, self.off:self.off + n].bitcast(I32)
                self.off += n
                assert self.off <= ARENA, self.off
                return a

        def psb(i, j):
            return ps[i][:, j, :]

        def psbf(i, j):
            return ps[i][:, j, :].bitcast(BF16)

        P.dma("sp", ident_f[:], ident, w=["ident_f"])
        P.dma("pool", ident_b[:], ident, w=["ident_b"])
        P.dma("sp", ut_f[:], utri, w=["ut_f"])
        P.dma("pool", bdg[:], bdiag, w=["bdg"])
        P.dma("sp", idxK_s[:], idxK, w=["idxK"])
        P.dma("sp", idxV_s[:], idxV, w=["idxV"])
        P.dma("sp", idxO_s[:], idxOwn, w=["idxO"])
        P.dma("sp", lamc_s[:], lamc, w=["lamc"])
        P.memset("dve", ones_f[:], 1.0, w=["ones_f"])
        P.memset("dve", epsb[:], EPS, w=["epsb"])
        P.dma("sp", cact[:], c_pk, w=["cact"])
        P.act(cact[:], cact[:], AF.Silu, r=["cact"], w=["cact"])
        P.dma("sp", XC, x_in, w=["XC"])
        def load_shared(l, only=None):
            specs = (("W_IN", W_IN, w_in_s, B_IN), ("W_ADA", W_ADA, w_ada_s, B_ADA), ("W_A", W_A, w_a_s, B_A),
                     ("W_B", W_B, w_b_s, B_B), ("W_O", W_O, w_o_s, B_O))
            if only is not None:
                specs = specs[only:only + 1]
            for nm, dst_, src_, bnc in specs:
                P.dma("sp", bnc[l], src_[l], w=["b%s%d" % (nm, l)])
            for nm, dst_, src_, bnc in specs:
                if nm in ("W_IN", "W_ADA"):
                    for p in range(8):
                        P.allgather(dst_[l][p * 128:(p + 1) * 128, :], bnc[l][p * 32:(p + 1) * 32, :], r=["b%s%d" % (nm, l)], w=["%s%d" % (nm, l)])
                else:
                    P.allgather(dst_[l], bnc[l], r=["b%s%d" % (nm, l)], w=["%s%d" % (nm, l)])

        load_shared(0)
        P.barrier()

        ring_i = [0]

        def ag_pieces(out_flat, in_flat, r0, npieces, r, w):
            for p in range(npieces):
                P.allgather(out_flat[p * 4 * r0:(p + 1) * 4 * r0, :], in_flat[p * r0:(p + 1) * r0, :], r=r, w=w)

        def ring_load(src_ap, key):
            i = ring_i[0] % 2
            ring_i[0] += 1
            t = wring[i][:].rearrange("p (k n) -> p k n", k=8)
            P.dma("pool", t, src_ap, r=[key] if key else [], w=["ring%d" % i])
            return t, "ring%d" % i

        def recip(eng, out, in_, r, w):
            return P.op(eng, lambda e: e.reciprocal(out, in_), r, w)

        def rms_rstd(ss, n, keys):
            P.act(ss, ss, AF.Sqrt, scale=1.0 / n, bias=epsb[:, 0:1], r=keys + ["epsb"], w=keys)
            recip("dve", ss, ss, keys, keys)

        for l in range(L):
            last = (l == L - 1)
            xdst = out if (last and final) else XC
            cv = Carver()
            cbc = cv.f32(8 * 128, ("p (k m) -> p k m", dict(k=8)))
            bada = cv.f32(6 * D)
            gtmp = cv.f32(4 * D, ("p (a n) -> p a n", dict(a=4)))
            wf = [cv.f32(8 * 512, ("p (k n) -> p k n", dict(k=8))) for _ in range(2)]
            for k in range(8):
                P.ts("dve", cbc[:, k, :], ones_f[:], cact[:, k:k + 1], ALU.mult, r=["ones_f", "cact"], w=["cbc"])
            P.dma("sp", bada, b_ada[l].partition_broadcast(128), w=["bada"])
            for a, gv in enumerate((g_pre_mix, g_post_mix, g_pre_ffn, g_post_ffn)):
                P.dma("sp", gtmp[:, a, :], gv[l].partition_broadcast(128), w=["gtmp%d" % a])
            wada_v = W_ADA[l].rearrange("(k p) n -> p k n", p=128)
            for cb in range(12):
                P.dma("sp", wf[cb % 2], wada_v[:, :, cb * 512:(cb + 1) * 512], r=["W_ADA%d" % l], w=["wf%d" % (cb % 2)])
                for k in range(8):
                    P.mm(psb(0, cb % 2), cbc[:, k, :], wf[cb % 2][:, k, :], start=(k == 0), stop=(k == 7),
                         r=["cbc", "wf%d" % (cb % 2)], w=["ps0_%d" % (cb % 2)])
                slot, half = cb // 2, cb % 2
                P.tt("dve", vec[:, slot, half * 512:(half + 1) * 512], psb(0, cb % 2), bada[:, cb * 512:(cb + 1) * 512],
                     ALU.add, r=["ps0_%d" % (cb % 2), "bada"], w=["vec%d" % slot])
            P.stt("dve", vec[:, 1, :], vec[:, 1, :], 1.0, gtmp[:, 0, :], ALU.add, ALU.mult, r=["vec1", "gtmp0"], w=["vec1"])
            P.tt("dve", vec[:, 2, :], vec[:, 2, :], gtmp[:, 1, :], ALU.mult, r=["vec2", "gtmp1"], w=["vec2"])
            P.stt("dve", vec[:, 4, :], vec[:, 4, :], 1.0, gtmp[:, 2, :], ALU.add, ALU.mult, r=["vec4", "gtmp2"], w=["vec4"])
            P.tt("dve", vec[:, 5, :], vec[:, 5, :], gtmp[:, 3, :], ALU.mult, r=["vec5", "gtmp3"], w=["vec5"])
            P.dma("sp", lamt[:].rearrange("p a n -> p (a n)"), lamv[l].rearrange("a n -> (a n)").partition_broadcast(128), w=["lamt"])
            P.tt("dve", lamt[:, 0, :], lamt[:, 0, :], lamt[:, 1, :], ALU.mult, r=["lamt"], w=["lamt"])
            P.tt("dve", lamt[:, 2, :], lamt[:, 2, :], lamt[:, 3, :], ALU.mult, r=["lamt"], w=["lamt"])
            P.red("dve", lamr[:, 0:1], lamt[:, 0, :], ALU.add, r=["lamt"], w=["lamr"])
            P.red("dve", lamr[:, 1:2], lamt[:, 2, :], ALU.add, r=["lamt"], w=["lamr"])
            P.act(lamr[:, 0:2], lamr[:, 0:2], AF.Exp, r=["lamr"], w=["lamr"])
            P.tt("dve", lamr[:, 2:3], lamr[:, 1:2], lamr[:, 0:1], ALU.subtract, r=["lamr"], w=["lamr"])
            P.tt("dve", nlam[:], lamr[:, 2:3], lamc_s[:, 2 * l:2 * l + 1], ALU.subtract, r=["lamr", "lamc"], w=["nlam"])
            P.dma("sp", gsc[:], g_subln[l].rearrange("(p o) -> p o", o=1), w=["gsc"])
            P.ts("dve", gsc[:], gsc[:], lamc_s[:, 2 * l + 1:2 * l + 2], ALU.mult, r=["gsc", "lamc"], w=["gsc"])
            P.dma("sp", wr_s[:], w_r[l].rearrange("(k p) e -> p k e", p=128), w=["wr_s"])
            P.dma("sp", br_s[:], b_r[l].partition_broadcast(128), w=["br_s"])
            if "vec" in dbg and l == 0:
                tv = tap("vec", [128, 6, D])
                P.dma("sp", tv, vec[:], r=["vec%d" % i for i in range(6)])
                tl = tap("nlam", [128, 1])
                P.dma("sp", tl, nlam[:], r=["nlam"])
            P.barrier()

            if "mix_a" in phases:
                cv = Carver()
                hT = cv.bf(8 * TOK, ("p (k t) -> p k t", dict(k=8)))
                uT = cv.bf(8 * TOK, ("p (k t) -> p k t", dict(k=8)))
                vgn = [cv.bf(D) for _ in range(2)]
                lnv = cv.f32(2 * D, ("p (a n) -> p a n", dict(a=2)))
                bsp = cv.f32(8 * 128)
                wst = cv.bf(8 * 128, ("p (g t) -> p g t", dict(g=8)))
                xt = [cv.f32(D) for _ in range(2)]
                t1 = [cv.f32(D) for _ in range(2)]
                hb = [cv.bf(D) for _ in range(2)]
                stg = [cv.f32(512) for _ in range(3)]
                stb = [cv.bf(TOK) for _ in range(2)]
                st16 = [cv.bf(512) for _ in range(3)]
                ss = cv.f32(8)
                P.dma("sp", lnv[:, 0, :], ln_v_g[l].partition_broadcast(128), w=["lnv"])
                P.dma("sp", lnv[:, 1, :], ln_v_b[l].partition_broadcast(128), w=["lnv"])
                P.dma("sp", bsp, b_sp[l].partition_broadcast(128), w=["bsp"])
                P.dma("pool", wst, wsT[l], w=["wst"])
                win_v = W_IN[l].rearrange("(k p) n -> p k n", p=128)
                nxt = ring_load(win_v[:, :, 0:D], "W_IN%d" % l)
                for t in range(NT):
                    b2 = t % 2
                    P.dma("sp", xt[b2], XC[t * 128:(t + 1) * 128, :], r=["XC"], w=["xt%d" % b2])
                    P.act(t1[b2], xt[b2], AF.Square, accum_out=ss[:, b2:b2 + 1], r=["xt%d" % b2], w=["t1%d" % b2, "ss%d" % b2])
                    rms_rstd(ss[:, b2:b2 + 1], D, ["ss%d" % b2])
                    P.stt("dve", t1[b2], xt[b2], ss[:, b2:b2 + 1], vec[:, 1, :], ALU.mult, ALU.mult,
                          r=["xt%d" % b2, "ss%d" % b2, "vec1"], w=["t1%d" % b2])
                    P.tt("dve", hb[b2], t1[b2], vec[:, 0, :], ALU.add, r=["t1%d" % b2, "vec0"], w=["hb%d" % b2])
                    for k in range(8):
                        P.tr(psbf(1, b2)[:, k * 128:(k + 1) * 128], hb[b2][:, k * 128:(k + 1) * 128], ident_b[:],
                             r=["hb%d" % b2, "ident_b"], w=["ps1_%d" % b2])
                    P.cp("act" if t % 2 else "dve", hT[:, :, t * 128:(t + 1) * 128],
                         psbf(1, b2).rearrange("p (k t) -> p k t", k=8), r=["ps1_%d" % b2], w=["hT"])
                if "hT" in dbg and l == 0:
                    th = tap("hT", [128, 8, TOK], BF16)
                    P.dma("sp", th, hT, r=["hT"])
                pi = [0]

                def nextps():
                    i = pi[0] % 4
                    pi[0] += 1
                    return 2 + i // 2, i % 2, "ps%d_%d" % (2 + i // 2, i % 2)

                for sec in range(7):
                    wt, wkey = nxt
                    if sec < 6:
                        nxt = ring_load(win_v[:, :, (sec + 1) * D:(sec + 2) * D], "W_IN%d" % l)
                    else:
                        nxt = ring_load(W_B[l].rearrange("(k p) n -> p k n", p=128), "W_B%d" % l)
                    if sec in (0, 1):
                        dst = QT if sec == 0 else KT_loc
                        dkey = "QT" if sec == 0 else "KT_loc"
                        for fc in range(8):
                            sbuf_i = fc % 2
                            for tg in range(4):
                                a, b_, pk = nextps()
                                for k in range(8):
                                    P.mm(psb(a, b_), wt[:, k, fc * 128:(fc + 1) * 128], hT[:, k, tg * 512:(tg + 1) * 512],
                                         start=(k == 0), stop=(k == 7), r=[wkey, "hT"], w=[pk])
                                osl = stb[sbuf_i][:, tg * 512:(tg + 1) * 512]
                                if sec == 0 and tg % 2:
                                    P.ts("dve", osl, psb(a, b_), 0.125, ALU.mult, r=[pk], w=["stb%d" % sbuf_i])
                                elif sec == 0:
                                    P.act(osl, psb(a, b_), AF.Copy, scale=0.125, r=[pk], w=["stb%d" % sbuf_i])
                                else:
                                    P.cp("dve" if tg % 2 else "act", osl, psb(a, b_), r=[pk], w=["stb%d" % sbuf_i])
                            P.dma("sp", dst[fc * 128:(fc + 1) * 128, :], stb[sbuf_i], r=["stb%d" % sbuf_i], w=[dkey])
                    elif sec == 2:
                        for tt_ in range(NT):
                            for half in range(2):
                                a, b_, pk = nextps()
                                for k in range(8):
                                    P.mm(psb(a, b_), hT[:, k, tt_ * 128:(tt_ + 1) * 128], wt[:, k, half * 512:(half + 1) * 512],
                                         start=(k == 0), stop=(k == 7), r=[wkey, "hT"], w=[pk])
                                si = (tt_ * 2 + half) % 3
                                P.cp("dve" if half else "act", st16[si], psb(a, b_), r=[pk], w=["st16%d" % si])
                                P.dma("sp", V_loc[half * 4:(half + 1) * 4, tt_ * 128:(tt_ + 1) * 128, :].rearrange("h t e -> t h e"),
                                      st16[si].rearrange("p (h e) -> p h e", h=4), r=["st16%d" % si], w=["V_loc"])
                        ag_pieces(KT_all, KT_loc, 256, 4, ["KT_loc"], ["KT_all"])
                        for hh in range(NH):
                            P.allgather(V_all[hh], V_loc[hh], r=["V_loc"], w=["V_all"])
                    elif sec == 3:
                        for fc in range(8):
                            for tg in range(4):
                                a, b_, pk = nextps()
                                for k in range(8):
                                    P.mm(psb(a, b_), wt[:, k, fc * 128:(fc + 1) * 128], hT[:, k, tg * 512:(tg + 1) * 512],
                                         start=(k == 0), stop=(k == 7), r=[wkey, "hT"], w=[pk])
                                P.act(uT[:, fc, tg * 512:(tg + 1) * 512], psb(a, b_), AF.Gelu, r=[pk], w=["uT"])
                    elif sec == 4:
                        for tt_ in range(NT):
                            b2 = tt_ % 2
                            for half in range(2):
                                for k in range(8):
                                    P.mm(ps[2 + b2][:, half, :], hT[:, k, tt_ * 128:(tt_ + 1) * 128], wt[:, k, half * 512:(half + 1) * 512],
                                         start=(k == 0), stop=(k == 7), r=[wkey, "hT"], w=["ps%d_%d" % (2 + b2, half)])
                            pv = ps[2 + b2][:].rearrange("p a n -> p (a n)")
                            P.act(xt[b2], pv, AF.Gelu, accum_out=ss[:, 2 + b2:3 + b2],
                                  r=["ps%d_0" % (2 + b2), "ps%d_1" % (2 + b2)], w=["xt%d" % b2, "ss%d" % (2 + b2)])
                            P.act(t1[b2], xt[b2], AF.Square, accum_out=ss[:, 4 + b2:5 + b2], r=["xt%d" % b2], w=["t1%d" % b2, "ss%d" % (4 + b2)])
                            m_ = ss[:, 2 + b2:3 + b2]
                            v_ = ss[:, 4 + b2:5 + b2]
                            kk = ["ss%d" % (2 + b2), "ss%d" % (4 + b2)]
                            P.ts("dve", m_, m_, 1.0 / D, ALU.mult, r=kk, w=kk)
                            P.ts("dve", v_, v_, 1.0 / D, ALU.mult, r=kk, w=kk)
                            P.tt("dve", ss[:, 6 + b2:7 + b2], m_, m_, ALU.mult, r=kk, w=["ss%d" % (6 + b2)])
                            P.tt("dve", v_, v_, ss[:, 6 + b2:7 + b2], ALU.subtract, r=kk + ["ss%d" % (6 + b2)], w=kk)
                            P.act(v_, v_, AF.Sqrt, bias=epsb[:, 0:1], r=kk + ["epsb"], w=kk)
                            recip("dve", v_, v_, kk, kk)
                            P.tt("dve", m_, m_, v_, ALU.mult, r=kk, w=kk)
                            P.ts("dve", m_, m_, -1.0, ALU.mult, r=kk, w=kk)
                            P.ts("dve", t1[b2], xt[b2], v_, ALU.mult, m_, ALU.add, r=["xt%d" % b2] + kk, w=["t1%d" % b2])
                            P.tt("dve", t1[b2], t1[b2], lnv[:, 0, :], ALU.mult, r=["t1%d" % b2, "lnv"], w=["t1%d" % b2])
                            P.tt("dve", vgn[b2], t1[b2], lnv[:, 1, :], ALU.add, r=["t1%d" % b2, "lnv"], w=["vgn%d" % b2])
                            pm = ps[b2][:].rearrange("p a n -> p (a n)")
                            pmk = ["ps%d_0" % b2, "ps%d_1" % b2]
                            for g in range(8):
                                P.mm(pm[:, g * 128:(g + 1) * 128], vgn[b2][:, g * 128:(g + 1) * 128], wst[:, g, :],
                                     r=["vgn%d" % b2, "wst"], w=pmk)
                            P.tt("dve", t1[b2], pm, bsp, ALU.add, r=pmk + ["bsp"], w=["t1%d" % b2])
                            uv = uT[:, :, tt_ * 128:(tt_ + 1) * 128]
                            P.tt("pool", uv, t1[b2].rearrange("p (g t) -> p g t", g=8), uv, ALU.mult, r=["t1%d" % b2, "uT"], w=["uT"])
                        pi[0] = 0
                    else:
                        dst = SGA if sec == 5 else SGB
                        dkey = "SGA" if sec == 5 else "SGB"
                        for fc in range(8):
                            for tg in range(4):
                                a, b_, pk = nextps()
                                for k in range(8):
                                    P.mm(psb(a, b_), wt[:, k, fc * 128:(fc + 1) * 128], hT[:, k, tg * 512:(tg + 1) * 512],
                                         start=(k == 0), stop=(k == 7), r=[wkey, "hT"], w=[pk])
                                si = (fc * 4 + tg) % 3
                                P.act(stg[si], psb(a, b_), AF.Sigmoid, r=[pk], w=["stg%d" % si])
                                P.dma("sp", dst[fc * 128:(fc + 1) * 128, tg * 512:(tg + 1) * 512], stg[si], r=["stg%d" % si], w=[dkey])
                wt, wkey = nxt
                for fc in range(8):
                    for tg in range(4):
                        a, b_, pk = nextps()
                        for k in range(8):
                            P.mm(psb(a, b_), wt[:, k, fc * 128:(fc + 1) * 128], uT[:, k, tg * 512:(tg + 1) * 512],
                                 start=(k == 0), stop=(k == 7), r=[wkey, "uT"], w=[pk])
                        si = (fc * 4 + tg) % 3
                        P.cp("dve" if tg % 2 else "act", stg[si], psb(a, b_), r=[pk], w=["stg%d" % si])
                        P.dma("sp", YB[fc * 128:(fc + 1) * 128, tg * 512:(tg + 1) * 512], stg[si], r=["stg%d" % si], w=["YB"])
                if l == 0:
                    for nm, src, shp, dt in (("QT", QT, [D, TOK], BF16), ("KT_all", KT_all, [4 * D, TOK], BF16),
                                             ("YB", YB, [D, TOK], F32),
                                             ("SGA", SGA, [D, TOK], F32)):
                        if nm in dbg:
                            tp = tap(nm, shp, dt)
                            P.dma("sp", tp, src, r=[nm])
                P.barrier()

            if "attn" in phases:
                cv = Carver()
                KTa = cv.bf(2 * SEQ, ("p (m n) -> p m n", dict(m=2)))
                KTbuf = [[KTa[:, 0, :], KTa[:, 1, :]], [wring[0][:], wring[1][:]]]
                Vbuf = [cv.bf(64 * 128, ("p (j e) -> p j e", dict(j=64))) for _ in range(2)]
                Qbuf = [cv.bf(6 * TOK, ("p (m v n) -> p m v n", dict(m=2, v=3))) for _ in range(2)]
                PT = [cv.bf(1024) for _ in range(3)]
                ppair = [cv.bf(1024) for _ in range(2)]
                accD = cv.f32(1024)
                accP = cv.f32(1024)
                rz = [cv.f32(512) for _ in range(2)]
                o0 = cv.f32(512)
                o1 = cv.f32(512)
                ob = [cv.bf(512) for _ in range(2)]
                osq = o1
                rs_ = rz[0]

                def load_head(h):
                    hb_ = h % 2
                    for m in range(2):
                        hm = m * 8 + h
                        for i in range(4):
                            P.gather(KTbuf[hb_][m][0:64, i * TOK:(i + 1) * TOK], KT_all, idxK_s[:, i * 16 + hm:i * 16 + hm + 1], 4 * D - 1,
                                     r=["KT_all", "idxK"], w=["KT%d" % hb_])
                        P.dma("pool", KTbuf[hb_][m][64:69, :], kaug[h], w=["KT%d" % hb_])
                        for v in range(3):
                            P.dma("sp", Qbuf[hb_][0:64, m, v, :], QT[hm * 64:(hm + 1) * 64, :], r=["QT"], w=["Qv%d" % hb_])
                            P.dma("pool", Qbuf[hb_][64:69, m, v, :], qaug[h, v], w=["Qv%d" % hb_])
                    for j in range(64):
                        P.gather(Vbuf[hb_][:, j, :], V_all[h], idxV_s[:, j:j + 1], SEQ - 1,
                                 r=["V_all", "idxV"], w=["Vh%d" % hb_])

                load_head(0)
                for h in range(NH):
                    if h + 1 < NH:
                        load_head(h + 1)
                    if h == 6 and l + 1 < L:
                        load_shared(l + 1)
                    hb_ = h % 2
                    KTs = KTbuf[hb_]
                    Vh = Vbuf[hb_]
                    Qv = Qbuf[hb_]
                    kK, kV, kQ = "KT%d" % hb_, "Vh%d" % hb_, "Qv%d" % hb_
                    for g in range(4):
                        P.memset("dve", accD, 0.0, w=["accD"])
                        P.memset("dve", accP, 0.0, w=["accP"])

                        def qk(j):
                            sbuf_i = (0, 1, 3)[j % 3]
                            i, jb = j // 16, j % 16
                            for m in range(2):
                                pk = "ps%d_%d" % (sbuf_i, m)
                                lhs = KTs[m][0:69, j * 128:(j + 1) * 128]
                                if i == 0 and 4 * g <= jb <= 4 * g + 3:
                                    for qb in range(4):
                                        qs = slice(g * 512 + qb * 128, g * 512 + (qb + 1) * 128)
                                        o_ = ps[sbuf_i][:, m, qb * 128:(qb + 1) * 128]
                                        if 4 * g + qb > jb:
                                            P.mm(o_, lhs, Qv[0:69, m, 0, qs], r=[kK, kQ], w=[pk])
                                        elif 4 * g + qb < jb:
                                            P.mm(o_, lhs, Qv[0:69, m, 1, qs], r=[kK, kQ], w=[pk])
                                        else:
                                            P.mm(o_, lhs, Qv[0:69, m, 2, qs], start=True, stop=False, r=[kK, kQ], w=[pk])
                                            P.mm(o_, ident_b[:], bdg[:, h, :], start=False, stop=True, r=["ident_b", "bdg"], w=[pk])
                                else:
                                    v = 1 if (i == 0 and jb > 4 * g + 3) else 0
                                    P.mm(ps[sbuf_i][:, m, :], lhs, Qv[0:69, m, v, g * 512:(g + 1) * 512], r=[kK, kQ], w=[pk])

                        def rest(j):
                            sbuf_i = (0, 1, 3)[j % 3]
                            pb = j % 3
                            P.act(PT[pb], ps[sbuf_i][:].rearrange("p a n -> p (a n)"), AF.Exp,
                                  r=["ps%d_0" % sbuf_i, "ps%d_1" % sbuf_i], w=["PT%d" % pb])
                            if j % 2 == 1:
                                pq = (j // 2) % 2
                                P.tt("dve", ppair[pq], PT[(j - 1) % 3], PT[pb], ALU.add,
                                     r=["PT%d" % ((j - 1) % 3), "PT%d" % pb], w=["pp%d" % pq])
                                acc_, ak_ = (accD, "accD") if pq == 0 else (accP, "accP")
                                P.tt("dve", acc_, acc_, ppair[pq], ALU.add, r=["pp%d" % pq, ak_], w=[ak_])
                            for m in range(2):
                                P.mm(ps[2][:, m, :], Vh[:, j, :], PT[pb][:, m * 512:(m + 1) * 512], start=(j == 0), stop=(j == 63),
                                     r=[kV, "PT%d" % pb], w=["ps2_%d" % m])

                        qk(0)
                        qk(1)
                        for j in range(64):
                            if j + 2 < 64:
                                qk(j + 2)
                            rest(j)
                        for m in range(2):
                            P.mm(ps[3][:, m, :], ones_f[:], accD[:, m * 512:(m + 1) * 512], start=True, stop=False,
                                 r=["ones_f", "accD"], w=["ps3_%d" % m])
                            P.mm(ps[3][:, m, :], ones_f[:], accP[:, m * 512:(m + 1) * 512], start=False, stop=True,
                                 r=["ones_f", "accP"], w=["ps3_%d" % m])
                            P.op("dve", (lambda m=m: (lambda e: e.reciprocal(rz[m], ps[3][:, m, :])))(), r=["ps3_%d" % m], w=["rz%d" % m])
                        P.tt("dve", o0, ps[2][:, 0, :], rz[0], ALU.mult, r=["ps2_0", "rz0"], w=["o0"])
                        P.tt("dve", o1, ps[2][:, 1, :], rz[1], ALU.mult, r=["ps2_1", "rz1"], w=["o1"])
                        P.stt("dve", o0, o1, nlam[:, 0:1], o0, ALU.mult, ALU.add, r=["o0", "o1", "nlam"], w=["o0"])
                        P.act(osq, o0, AF.Square, r=["o0"], w=["o1"])
                        P.mm(ps[3][:, 0, :], ones_f[:], osq, r=["ones_f", "o1"], w=["ps3_0"])
                        P.act(rs_, ps[3][:, 0, :], AF.Sqrt, scale=1.0 / 128, bias=epsb[:, 0:1], r=["ps3_0", "epsb"], w=["rz0"])
                        recip("dve", rs_, rs_, ["rz0"], ["rz0"])
                        bi = (h * 4 + g) % 2
                        P.stt("dve", ob[bi], o0, gsc[:, 0:1], rs_, ALU.mult, ALU.mult, r=["o0", "gsc", "rz0"], w=["ob%d" % bi])
                        P.dma("sp", OT[h * 128:(h + 1) * 128, g * 512:(g + 1) * 512], ob[bi], r=["ob%d" % bi], w=["OT"])
                if "OT" in dbg and l == 0:
                    tp = tap("OT", [D, TOK], BF16)
                    P.dma("sp", tp, OT, r=["OT"])
                P.barrier()

            if "mix_b" in phases:
                cv = Carver()
                oTs = cv.bf(8 * TOK, ("p (k t) -> p k t", dict(k=8)))
                mT = cv.bf(8 * TOK, ("p (k t) -> p k t", dict(k=8)))
                la = [cv.f32(512) for _ in range(2)]
                lb = [cv.f32(512) for _ in range(2)]
                ly = [cv.f32(512) for _ in range(2)]
                m1 = [cv.f32(512) for _ in range(2)]
                m2 = [cv.f32(512) for _ in range(2)]
                xt = [cv.f32(D) for _ in range(2)]
                xn = [cv.f32(D) for _ in range(2)]
                t1_ = cv.f32(D)
                t1 = [t1_, t1_]
                h2 = [cv.f32(D) for _ in range(2)]
                h2T_ = cv.f32(8 * 128, ("p (k t) -> p k t", dict(k=8)))
                h2T = [h2T_, h2T_]
                lg = cv.f32(2 * NE, ("p (a e) -> p a e", dict(a=2)))
                ss = cv.f32(12)
                wa, wak = ring_load(W_A[l].rearrange("(k p) n -> p k n", p=128), "W_A%d" % l)
                wo, wok = ring_load(W_O[l].rearrange("(k p) n -> p k n", p=128), "W_O%d" % l)
                for k in range(8):
                    P.dma("sp", oTs[:, k, :], OT[k * 128:(k + 1) * 128, :], r=["OT"], w=["oTs"])
                cnt = 0
                for fc in range(8):
                    for tg in range(4):
                        b2 = cnt % 2
                        cnt += 1
                        rs = slice(fc * 128, (fc + 1) * 128)
                        cs = slice(tg * 512, (tg + 1) * 512)
                        P.dma("sp", la[b2], SGA[rs, cs], r=["SGA"], w=["la%d" % b2])
                        P.dma("sp", lb[b2], SGB[rs, cs], r=["SGB"], w=["lb%d" % b2])
                        P.dma("sp", ly[b2], YB[rs, cs], r=["YB"], w=["ly%d" % b2])
                        for k in range(8):
                            P.mm(psb(0, b2), wa[:, k, rs], oTs[:, k, cs], start=(k == 0), stop=(k == 7), r=[wak, "oTs"], w=["ps0_%d" % b2])
                        P.tt("dve", m1[b2], psb(0, b2), la[b2], ALU.mult, r=["ps0_%d" % b2, "la%d" % b2], w=["m1%d" % b2])
                        P.tt("pool", m2[b2], lb[b2], ly[b2], ALU.mult, r=["lb%d" % b2, "ly%d" % b2], w=["m2%d" % b2])
                        P.tt("dve", mT[:, fc, cs], m1[b2], m2[b2], ALU.add, r=["m1%d" % b2, "m2%d" % b2], w=["mT"])
                for t in range(NT):
                    b2 = t % 2
                    tsl = slice(t * 128, (t + 1) * 128)
                    P.dma("sp", xt[b2], XC[tsl, :], r=["XC"], w=["xt%d" % b2])
                    for half in range(2):
                        for k in range(8):
                            P.mm(ps[1][:, half, :], mT[:, k, tsl], wo[:, k, half * 512:(half + 1) * 512], start=(k == 0), stop=(k == 7),
                                 r=[wok, "mT"], w=["ps1_%d" % half])
                    pv = ps[1][:].rearrange("p a n -> p (a n)")
                    s0 = ss[:, b2:b2 + 1]
                    P.act(t1[b2], pv, AF.Square, accum_out=s0, r=["ps1_0", "ps1_1"], w=["t1c", "ssa%d" % b2])
                    rms_rstd(s0, D, ["ssa%d" % b2])
                    P.stt("dve", t1[b2], pv, s0, vec[:, 2, :], ALU.mult, ALU.mult, r=["ps1_0", "ps1_1", "ssa%d" % b2, "vec2"], w=["t1c"])
                    P.tt("dve", xn[b2], t1[b2], xt[b2], ALU.add, r=["t1c", "xt%d" % b2], w=["xn%d" % b2])
                    P.dma("sp", XC[tsl, :], xn[b2], r=["xn%d" % b2], w=["XC"])
                    s1 = ss[:, 2 + b2:3 + b2]
                    P.act(t1[b2], xn[b2], AF.Square, accum_out=s1, r=["xn%d" % b2], w=["t1c", "ssb%d" % b2])
                    rms_rstd(s1, D, ["ssb%d" % b2])
                    P.stt("dve", t1[b2], xn[b2], s1, vec[:, 4, :], ALU.mult, ALU.mult, r=["xn%d" % b2, "ssb%d" % b2, "vec4"], w=["t1c"])
                    P.tt("dve", h2[b2], t1[b2], vec[:, 3, :], ALU.add, r=["t1c", "vec3"], w=["h2%d" % b2])
                    P.dma("sp", H2_loc[tsl, :], h2[b2], r=["h2%d" % b2], w=["H2_loc%d" % (t // 2)])
                    if t % 2 == 1:
                        p_ = t // 2
                        P.allgather(H2_all[p_ * 1024:(p_ + 1) * 1024, :], H2_loc[p_ * 256:(p_ + 1) * 256, :], r=["H2_loc%d" % p_], w=["H2_all"])
                    for k in range(8):
                        P.tr(ps[2][:].rearrange("p a n -> p (a n)")[:, k * 128:(k + 1) * 128], h2[b2][:, k * 128:(k + 1) * 128], ident_f[:],
                             r=["h2%d" % b2, "ident_f"], w=["ps2_0", "ps2_1"])
                    P.cp("act", h2T[b2], ps[2][:].rearrange("p a (k t) -> p (a k) t", t=128), r=["ps2_0", "ps2_1"], w=["h2Tc"])
                    for k in range(8):
                        P.mm(ps[3][:, 0, 0:NE], h2T[b2][:, k, :], wr_s[:, k, :], start=(k == 0), stop=(k == 7),
                             r=["h2Tc", "wr_s"], w=["ps3_0"])
                    P.tt("dve", lg[:, 0, :], ps[3][:, 0, 0:NE], br_s[:], ALU.add, r=["ps3_0", "br_s"], w=["lg"])
                    s2 = ss[:, 4 + b2:5 + b2]
                    s3 = ss[:, 6 + b2:7 + b2]
                    P.red("dve", s2, lg[:, 0, :], ALU.max, r=["lg"], w=["ssc%d" % b2])
                    P.ts("dve", s2, s2, -1.0, ALU.mult, r=["ssc%d" % b2], w=["ssc%d" % b2])
                    P.act(lg[:, 1, :], lg[:, 0, :], AF.Exp, bias=s2, accum_out=s3, r=["lg", "ssc%d" % b2], w=["lg", "ssd%d" % b2])
                    P.op("dve", (lambda s3=s3: (lambda e: e.reciprocal(s3, s3)))(), r=["ssd%d" % b2], w=["ssd%d" % b2])
                    P.ts("dve", aff_own[:, t, :], lg[:, 1, :], s3, ALU.mult, r=["lg", "ssd%d" % b2], w=["aff_own"])
                P.dma("sp", AFF_loc.rearrange("(t p) e -> p t e", p=128), aff_own[:], r=["aff_own"], w=["AFF_loc"])
                P.allgather(AFF_all, AFF_loc, r=["AFF_loc"], w=["AFF_all"])
                if l == 0:
                    if "XC1" in dbg:
                        tp = tap("XC1", [TOK, D])
                        P.dma("sp", tp, XC, r=["XC"])
                    if "AFF_all" in dbg:
                        tp = tap("AFF_all", [SEQ, NE])
                        P.dma("sp", tp, AFF_all, r=["AFF_all"])
                P.barrier()

            if "moe_r" in phases:
                cv = Carver()
                affA = cv.f32(64 * NE, ("p (j e) -> p j e", dict(j=64)))
                msk = cv.f32(64 * NE, ("p (j e) -> p j e", dict(j=64)))
                lo = cv.f32(NE)
                hi = cv.f32(NE)
                mid = cv.f32(NE)
                dd = cv.f32(NE)
                ge = cv.f32(NE)
                cpart = cv.f32(NE)
                tot = cv.f32(64 * NE, ("p (j e) -> p j e", dict(j=64)))
                cs0 = cv.f32(64 * NE, ("p (j e) -> p j e", dict(j=64)))
                cs1 = cv.f32(64 * NE, ("p (j e) -> p j e", dict(j=64)))
                pos = cv.f32(64 * NE, ("p (j e) -> p j e", dict(j=64)))
                idf = cv.f32(64 * 2 * NE, ("p (j c) -> p j c", dict(j=64)))
                selt = cv.f32(EOWN * 64 * NE, ("p (i n) -> p i n", dict(i=EOWN)))
                tmp = cv.f32(64 * NE, ("p (j e) -> p j e", dict(j=64)))
                pI = cv.f32(EOWN * 64, ("p (i j) -> p i j", dict(i=EOWN)))
                mI = cv.f32(EOWN * 64, ("p (i j) -> p i j", dict(i=EOWN)))
                pIi = cv.i32(EOWN * 64).rearrange("p (i j) -> p i j", i=EOWN)
                hx = [cv.f32(D) for _ in range(8)]
                P.dma("sp", affA, AFF_all.rearrange("(j t) e -> t j e", t=128), r=["AFF_all"], w=["affA"])
                P.dma("sp", selt, selb, w=["selt"])
                P.memset("dve", lo, 0.0, w=["lo"])
                P.memset("dve", hi, 1.0, w=["hi"])
                kb = ["lo", "hi", "mid", "dd", "ge"]

                def mask_ge(thr, key):
                    for e_ in range(NE):
                        P.ts("dve", msk[:, :, e_], affA[:, :, e_], thr[:, e_:e_ + 1], ALU.is_ge, r=["affA", key], w=["msk"])

                for it in range(NBIS):
                    P.tt("dve", mid, lo, hi, ALU.add, r=kb, w=kb)
                    P.ts("dve", mid, mid, 0.5, ALU.mult, r=kb, w=kb)
                    mask_ge(mid, "mid")
                    P.red("dve", cpart, msk[:].rearrange("p j e -> p e j"), ALU.add, r=["msk"], w=["cpart"])
                    P.mm(ps[0][:, 0, 0:NE], ones_f[:], cpart, r=["ones_f", "cpart"], w=["ps0_0"])
                    P.ts("dve", ge, ps[0][:, 0, 0:NE], CAP - 0.5, ALU.is_ge, r=["ps0_0"], w=kb)
                    P.tt("dve", dd, mid, lo, ALU.subtract, r=kb, w=kb)
                    P.tt("dve", hi, hi, dd, ALU.subtract, r=kb, w=kb)
                    P.tt("dve", dd, dd, ge, ALU.mult, r=kb, w=kb)
                    P.tt("dve", lo, lo, dd, ALU.add, r=kb, w=kb)
                    P.tt("dve", hi, hi, dd, ALU.add, r=kb, w=kb)
                mask_ge(lo, "lo")
                mflat = msk[:].rearrange("p j e -> p (j e)")
                for half in range(2):
                    P.mm(ps[1][:, half, :], ut_f[:], mflat[:, half * 512:(half + 1) * 512], r=["ut_f", "msk"], w=["ps1_%d" % half])
                    P.mm(ps[2][:, half, :], ones_f[:], mflat[:, half * 512:(half + 1) * 512], r=["ones_f", "msk"], w=["ps2_%d" % half])
                P.cp("dve", tot[:].rearrange("p j e -> p (j e)"), ps[2][:].rearrange("p a n -> p (a n)"), r=["ps2_0", "ps2_1"], w=["tot"])
                P.cp("dve", cs0[:].rearrange("p j e -> p (j e)"), tot[:].rearrange("p j e -> p (j e)"), r=["tot"], w=["cs0"])
                src, dst, sk, dk = cs0, cs1, "cs0", "cs1"
                sh = 1
                while sh < 64:
                    P.cp("dve", dst[:, 0:sh, :], src[:, 0:sh, :], r=[sk], w=[dk])
                    P.tt("dve", dst[:, sh:64, :], src[:, sh:64, :], src[:, 0:64 - sh, :], ALU.add, r=[sk], w=[dk])
                    src, dst, sk, dk = dst, src, dk, sk
                    sh *= 2
                P.tt("dve", pos[:].rearrange("p j e -> p (j e)"), src[:].rearrange("p j e -> p (j e)"), tot[:].rearrange("p j e -> p (j e)"),
                     ALU.subtract, r=[sk, "tot"], w=["pos"])
                P.tt("dve", pos[:].rearrange("p j e -> p (j e)"), pos[:].rearrange("p j e -> p (j e)"), ps[1][:].rearrange("p a n -> p (a n)"),
                     ALU.add, r=["pos", "ps1_0", "ps1_1"], w=["pos"])
                P.ts("dve", tmp[:].rearrange("p j e -> p (j e)"), pos[:].rearrange("p j e -> p (j e)"), CAP - 0.5, ALU.is_le, r=["pos"], w=["tmp"])
                P.tt("dve", msk[:].rearrange("p j e -> p (j e)"), msk[:].rearrange("p j e -> p (j e)"), tmp[:].rearrange("p j e -> p (j e)"),
                     ALU.mult, r=["msk", "tmp"], w=["msk"])
                for i in range(EOWN):
                    P.tt("dve", tmp[:].rearrange("p j e -> p (j e)"), pos[:].rearrange("p j e -> p (j e)"), selt[:, i, :], ALU.mult,
                         r=["pos", "selt"], w=["tmp"])
                    P.red("dve", pI[:, i, :], tmp, ALU.add, r=["tmp"], w=["pI"])
                    P.tt("dve", tmp[:].rearrange("p j e -> p (j e)"), msk[:].rearrange("p j e -> p (j e)"), selt[:, i, :], ALU.mult,
                         r=["msk", "selt"], w=["tmp"])
                    P.red("dve", mI[:, i, :], tmp, ALU.add, r=["tmp"], w=["mI"])
                pf = pI[:].rearrange("p i j -> p (i j)")
                mf = mI[:].rearrange("p i j -> p (i j)")
                P.ts("dve", pf, pf, -BIG, ALU.add, r=["pI"], w=["pI"])
                P.tt("dve", pf, pf, mf, ALU.mult, r=["pI", "mI"], w=["pI"])
                for i in range(EOWN):
                    P.ts("dve", pI[:, i, :], pI[:, i, :], BIG + i * CAP, ALU.add, r=["pI"], w=["pI"])
                P.cp("dve", pIi[:].rearrange("p i j -> p (i j)"), pf, r=["pI"], w=["pIi"])
                pflat = pos[:].rearrange("p j e -> p (j e)")
                tflat = tmp[:].rearrange("p j e -> p (j e)")
                c0f = cs0[:].rearrange("p j e -> p (j e)")
                P.ts("dve", c0f, pflat, 255.5, ALU.is_ge, r=["pos"], w=["cs0"])
                for thr_ in (511.5, 767.5):
                    P.ts("dve", tflat, pflat, thr_, ALU.is_ge, r=["pos"], w=["tmp"])
                    P.tt("dve", c0f, c0f, tflat, ALU.add, r=["cs0", "tmp"], w=["cs0"])
                P.stt("dve", c0f, c0f, 768.0, pflat, ALU.mult, ALU.add, r=["cs0", "pos"], w=["cs0"])
                for e_ in range(NE):
                    base_e = (e_ % 4) * 4096 + (e_ // 4) * 256
                    P.ts("dve", idf[:, :, e_], cs0[:, :, e_], float(base_e) - BIG, ALU.add, r=["cs0"], w=["idf"])
                P.tt("dve", idf[:, :, 0:NE], idf[:, :, 0:NE], msk, ALU.mult, r=["idf", "msk"], w=["idf"])
                P.ts("dve", idf[:, :, 0:NE], idf[:, :, 0:NE], BIG, ALU.add, r=["idf"], w=["idf"])
                P.cp("dve", idf[:, :, NE:2 * NE], msk, r=["msk"], w=["idf"])
                P.dma("sp", IDXF.rearrange("(j t) c -> t j c", t=128), idf, r=["idf"], w=["IDXF"])
                for j in range(64):
                    hb_ = j % 8
                    tl_ = (j % 16) * 128
                    row0 = (tl_ // 256) * 1024 + (j // 16) * 256 + tl_ % 256
                    P.dma("sp", hx[hb_], H2_all[row0:row0 + 128, :], r=["H2_all"], w=["hx%d" % hb_])
                    for i in range(EOWN):
                        P.scatter(XG, pIi[:, i, j:j + 1], hx[hb_], EOWN * CAP - 1, r=["hx%d" % hb_, "pIi"], w=["XG"])
                if l == 0:
                    if "IDXF" in dbg:
                        tp = tap("IDXF", [SEQ, 2 * NE])
                        P.dma("sp", tp, IDXF, r=["IDXF"])
                    if "XG" in dbg:
                        tp = tap("XG", [EOWN * CAP, D])
                        P.dma("sp", tp, XG, r=["XG"])
                P.barrier()

            if "moe_e" in phases:
                cv = Carver()
                XT = cv.bf(8 * CAP, ("p (k s) -> p k s", dict(k=8)))
                hid = cv.bf(16 * CAP, ("p (f s) -> p f s", dict(f=16)))
                xg = [cv.bf(D) for _ in range(2)]
                sg_ = [cv.f32(512) for _ in range(2)]
                ys = [cv.f32(D) for _ in range(2)]
                wp = [cv.bf(8 * 512) for _ in range(6)]
                wpi = [0]

                def piece(src_ap, shape):
                    i = wpi[0] % 6
                    wpi[0] += 1
                    t = wp[i].rearrange(shape[0], **shape[1])
                    P.dma("pool", t, src_ap, w=["wp%d" % i])
                    return t, "wp%d" % i

                for i in range(n_own):
                    for sbk in range(8):
                        b2 = sbk % 2
                        P.dma("pool", xg[b2], XG[i * CAP + sbk * 128:i * CAP + (sbk + 1) * 128, :], r=["XG"], w=["xg%d" % b2])
                        for k in range(8):
                            P.tr(psbf(3, b2)[:, k * 128:(k + 1) * 128], xg[b2][:, k * 128:(k + 1) * 128], ident_b[:],
                                 r=["xg%d" % b2, "ident_b"], w=["ps3_%d" % b2])
                        P.cp("dve", XT[:, :, sbk * 128:(sbk + 1) * 128], psbf(3, b2).rearrange("p (k t) -> p k t", k=8),
                             r=["ps3_%d" % b2], w=["XT"])
                    wgv = w_ge[l, i].rearrange("(k p) f -> p k f", p=128)
                    wuv = w_ue[l, i].rearrange("(k p) f -> p k f", p=128)
                    wdv = w_de[l, i].rearrange("(f p) n -> p f n", p=128)
                    cnt = 0
                    for fb in range(4):
                        wg_, wgk = piece(wgv[:, :, fb * 512:(fb + 1) * 512], ("p (k n) -> p k n", dict(k=8)))
                        wu_, wuk = piece(wuv[:, :, fb * 512:(fb + 1) * 512], ("p (k n) -> p k n", dict(k=8)))
                        for fc in range(4):
                            for sgi in range(2):
                                b2 = cnt % 2
                                cnt += 1
                                ssl = slice(sgi * 512, (sgi + 1) * 512)
                                for k in range(8):
                                    P.mm(ps[b2][:, 0, :], wg_[:, k, fc * 128:(fc + 1) * 128], XT[:, k, ssl], start=(k == 0), stop=(k == 7),
                                         r=[wgk, "XT"], w=["ps%d_0" % b2])
                                for k in range(8):
                                    P.mm(ps[b2][:, 1, :], wu_[:, k, fc * 128:(fc + 1) * 128], XT[:, k, ssl], start=(k == 0), stop=(k == 7),
                                         r=[wuk, "XT"], w=["ps%d_1" % b2])
                                P.act(sg_[b2], ps[b2][:, 0, :], AF.Silu, r=["ps%d_0" % b2], w=["sg%d" % b2])
                                P.tt("dve", hid[:, fb * 4 + fc, ssl], sg_[b2], ps[b2][:, 1, :], ALU.mult, r=["sg%d" % b2, "ps%d_1" % b2], w=["hid"])
                    wds = [piece(wdv[:, q * 4:(q + 1) * 4, :], ("p (f n) -> p f n", dict(f=4))) for q in range(4)]
                    for sbk in range(8):
                        b2 = sbk % 2
                        for half in range(2):
                            for f in range(16):
                                wd_, wdk = wds[f // 4]
                                P.mm(ps[2][:, half, :], hid[:, f, sbk * 128:(sbk + 1) * 128], wd_[:, f % 4, half * 512:(half + 1) * 512],
                                     start=(f == 0), stop=(f == 15), r=["hid", wdk], w=["ps2_%d" % half])
                        P.cp("act", ys[b2], ps[2][:].rearrange("p a n -> p (a n)"), r=["ps2_0", "ps2_1"], w=["ys%d" % b2])
                        P.dma("sp", Y_loc[i * CAP + sbk * 128:i * CAP + (sbk + 1) * 128, :], ys[b2], r=["ys%d" % b2], w=["Y_loc%d" % i])
                    for p in range(4 * i, 4 * i + 4):
                        P.allgather(Y_all[p * 1024:(p + 1) * 1024, :], Y_loc[p * 256:(p + 1) * 256, :], r=["Y_loc%d" % i], w=["Y_all"])
                if "Y_loc" in dbg and l == 0:
                    tp = tap("Y_loc", [EOWN * CAP, D])
                    P.dma("sp", tp, Y_loc, r=["Y_loc%d" % i for i in range(n_own)])
                P.barrier()

            if "moe_c" in phases:
                cv = Carver()
                own = cv.f32(NT * 2 * NE, ("p (t c) -> p t c", dict(t=NT)))
                gidx = cv.i32(NT * NE).rearrange("p (t e) -> p t e", t=NT)
                gate = cv.f32(NT * NE, ("p (t e) -> p t e", dict(t=NT)))
                G = [cv.f32(D) for _ in range(4)]
                accA = cv.f32(D)
                accB = cv.f32(D)
                xt = [cv.f32(D) for _ in range(2)]
                t1 = cv.f32(D)
                ss = cv.f32(4)
                for q in range(4):
                    P.memset("dve", G[q], 0.0, w=["G%d" % q])
                for t in range(NT):
                    P.gather(own[:, t, :], IDXF, idxO_s[:, t:t + 1], SEQ - 1, r=["IDXF", "idxO"], w=["own"])
                P.cp("dve", gidx, own[:, :, 0:NE], r=["own"], w=["gidx"])
                P.tt("dve", gate, own[:, :, NE:2 * NE], aff_own[:], ALU.mult, r=["own", "aff_own"], w=["gate"])
                for t in range(NT):
                    b2 = t % 2
                    tsl = slice(t * 128, (t + 1) * 128)
                    P.dma("sp", xt[b2], XC[tsl, :], r=["XC"], w=["xt%d" % b2])
                    for e_ in range(NE):
                        q = e_ % 4
                        P.gather(G[q], Y_all, gidx[:, t, e_:e_ + 1], NE * CAP - 1, r=["Y_all", "gidx"], w=["G%d" % q])
                        eng, acc, ak = ("dve", accA, "accA") if e_ % 2 == 0 else ("dve", accB, "accB")
                        if e_ < 2:
                            P.ts(eng, acc, G[q], gate[:, t, e_:e_ + 1], ALU.mult, r=["G%d" % q, "gate"], w=[ak])
                        elif eng == "dve":
                            P.stt(eng, acc, G[q], gate[:, t, e_:e_ + 1], acc, ALU.mult, ALU.add, r=["G%d" % q, "gate", ak], w=[ak])
                        else:
                            P.ts(eng, G[q], G[q], gate[:, t, e_:e_ + 1], ALU.mult, r=["G%d" % q, "gate"], w=["G%d" % q])
                            P.tt(eng, acc, acc, G[q], ALU.add, r=["G%d" % q, ak], w=[ak])
                    P.tt("dve", accA, accA, accB, ALU.add, r=["accA", "accB"], w=["accA"])
                    s0 = ss[:, b2:b2 + 1]
                    P.act(t1, accA, AF.Square, accum_out=s0, r=["accA"], w=["t1", "ss%d" % b2])
                    rms_rstd(s0, D, ["ss%d" % b2])
                    P.stt("dve", t1, accA, s0, vec[:, 5, :], ALU.mult, ALU.mult, r=["accA", "ss%d" % b2, "vec5"], w=["t1"])
                    P.tt("dve", xt[b2], t1, xt[b2], ALU.add, r=["t1", "xt%d" % b2], w=["xt%d" % b2])
                    P.dma("sp", xdst[tsl, :], xt[b2], r=["xt%d" % b2], w=["XC" if xdst is XC else "out"])
                P.barrier()
            elif last and final:
                P.dma("sp", out, XC, r=["XC"], w=["out"])
                P.barrier()

        with nc.Block() as block:
            P.emit(block)
    return nc, taps


def _const_tables(r):
    slopes = [2.0 ** (-(h + 1)) for h in range(NH)]
    ql = np.arange(TOK)
    q_hi = (ql // 64) * 64
    q_lo = ql % 64
    qaug = np.zeros((NH, 3, 5, TOK), np.float32)
    kaug = np.zeros((NH, 5, SEQ), np.float32)
    bdiag = np.zeros((128, NH, 128), np.float32)
    kl = np.arange(TOK)
    k_hi = (kl // 64) * 64
    k_lo = kl % 64
    dloc = np.abs(np.arange(128)[:, None] - np.arange(128)[None, :]).astype(np.float32)
    for h, c in enumerate(slopes):
        below = np.stack([c * q_hi, c * q_lo, np.ones(TOK), np.ones(TOK), np.ones(TOK)]).astype(np.float32)
        qaug[h, 0] = below
        qaug[h, 1] = -below
        qaug[h, 2] = 0.0
        for i in range(4):
            sl = slice(i * TOK, (i + 1) * TOK)
            if i == 0:
                sg, dl = 1.0, 0.0
            else:
                pr = (r + i) % 4
                delta = 2048.0 * (r - pr)
                sg = 1.0 if delta > 0 else -1.0
                dl = abs(delta)
            kaug[h, 0, sl] = -sg
            kaug[h, 1, sl] = -sg
            kaug[h, 2, sl] = sg * c * k_hi
            kaug[h, 3, sl] = sg * c * k_lo
            kaug[h, 4, sl] = -c * dl
        bdiag[:, h, :] = -c * dloc
    idxK = np.zeros((64, 64), np.int32)
    for i in range(4):
        for hm in range(16):
            idxK[:, i * 16 + hm] = (hm // 4) * 1024 + ((r + i) % 4) * 256 + (hm % 4) * 64 + np.arange(64)
    idxV = np.zeros((128, 64), np.int32)
    for j in range(64):
        idxV[:, j] = ((r + j // 16) % 4) * TOK + (j % 16) * 128 + np.arange(128)
    idxOwn = np.zeros((128, NT), np.int32)
    for t in range(NT):
        idxOwn[:, t] = r * TOK + t * 128 + np.arange(128)
    selb = np.zeros((128, EOWN, 64, NE), np.float32)
    for i in range(EOWN):
        selb[:, i, :, 4 * r + i] = 1.0
    return dict(qaug=qaug, kaug=kaug, bdiag=bdiag, idxK=idxK, idxV=idxV, idxOwn=idxOwn,
                selb=selb.reshape(128, EOWN, 64 * NE))


def make_in_maps(inp, layers, xs=None, n_own=EOWN, with_experts=True):
    L = len(layers)
    ls = list(layers)
    f = lambda a: np.ascontiguousarray(a, dtype=np.float32)
    ident = np.eye(128, dtype=np.float32)
    utri = np.triu(np.ones((128, 128), np.float32), k=1)
    lamc = np.zeros((128, 2 * L), np.float32)
    for i, l in enumerate(ls):
        li = 0.8 - 0.6 * math.exp(-0.3 * l)
        lamc[:, 2 * i] = li
        lamc[:, 2 * i + 1] = 1.0 - li
    lamv = np.stack([np.stack([inp["lam_q1"][l], inp["lam_k1"][l], inp["lam_q2"][l], inp["lam_k2"][l]]) for l in ls])
    wsT = np.stack([np.transpose(inp["w_spatial"][l], (2, 0, 1)) for l in ls])
    b_sp = np.stack([inp["b_spatial"][l].reshape(-1) for l in ls])
    maps = []
    for c in range(8):
        b, r = c // 4, c % 4
        rs = slice(r * 256, (r + 1) * 256)
        rp = (np.arange(8)[:, None] * 128 + r * 32 + np.arange(32)[None, :]).reshape(-1)
        m = dict(
            x=f(inp["x"][b, r * TOK:(r + 1) * TOK] if xs is None else xs[c]),
            c_pk=f(np.asarray(inp["c"][b]).reshape(8, 128).T),
            w_ada_s=f(np.stack([inp["w_ada"][l][rp] for l in ls])),
            w_in_s=f(np.stack([inp["w_in"][l][rp] for l in ls])),
            w_a_s=f(np.stack([inp["w_branch_a"][l][rs] for l in ls])),
            w_b_s=f(np.stack([inp["w_branch_b"][l][rs] for l in ls])),
            w_o_s=f(np.stack([inp["w_out"][l][rs] for l in ls])),
            b_ada=f(np.stack([inp["b_ada"][l] for l in ls])),
            g_pre_mix=f(np.stack([inp["g_pre_mix"][l] for l in ls])),
            g_post_mix=f(np.stack([inp["g_post_mix"][l] for l in ls])),
            g_pre_ffn=f(np.stack([inp["g_pre_ffn"][l] for l in ls])),
            g_post_ffn=f(np.stack([inp["g_post_ffn"][l] for l in ls])),
            ln_v_g=f(np.stack([inp["ln_v_g"][l] for l in ls])),
            ln_v_b=f(np.stack([inp["ln_v_b"][l] for l in ls])),
            g_subln=f(np.stack([inp["g_subln"][l] for l in ls])),
            lamv=f(lamv), lamc=lamc, wsT=f(wsT), b_sp=f(b_sp),
            w_r=f(np.stack([inp["w_router"][l] for l in ls])),
            b_r=f(np.stack([inp["b_router"][l] for l in ls])),
            ident=ident, utri=utri,
        )
        if with_experts:
            m["w_ge"] = f(np.stack([inp["w_gate_e"][l][4 * r:4 * r + n_own] for l in ls]))
            m["w_ue"] = f(np.stack([inp["w_up_e"][l][4 * r:4 * r + n_own] for l in ls]))
            m["w_de"] = f(np.stack([inp["w_down_e"][l][4 * r:4 * r + n_own] for l in ls]))
        m.update(_const_tables(r))
        maps.append(m)
    return maps


_PROG_CACHE = {}
FUSED = True


def _get_prog(L):
    if L not in _PROG_CACHE:
        lam_inits = [0.8 - 0.6 * math.exp(-0.3 * l) for l in range(L)]
        _PROG_CACHE[L] = build_program(L, lam_inits)[0]
    return _PROG_CACHE[L]


def kernel(**inputs):
    inp = {k: np.asarray(v) for k, v in inputs.items()}
    depth = 4
    if FUSED:
        nc = _get_prog(depth)
        maps = make_in_maps(inp, range(depth))
        res = run_bass_kernel_spmd(nc, maps, core_ids=list(range(8)))
        shards = [res.results[c]["out"] for c in range(8)]
    else:
        nc = _get_prog(1)
        shards = None
        for l in range(depth):
            maps = make_in_maps(inp, [l], xs=shards)
            res = run_bass_kernel_spmd(nc, maps, core_ids=list(range(8)))
            shards = [np.asarray(res.results[c]["out"]) for c in range(8)]
    outp = np.zeros((2, SEQ, D), np.float32)
    for c in range(8):
        b, r = c // 4, c % 4
        outp[b, r * TOK:(r + 1) * TOK] = shards[c]
    return outp
```
